# Optimizing a Trainium2 kernel written in Bass

```python
import jax, jax.numpy as jnp
from jax import lax
import numpy as np

D_MODEL = 1024
BATCH = 8
SEQ = 4096
DEPTH = 4

N_MIXERS = 2
N_GLA_LAYERS = (DEPTH + N_MIXERS - 1) // N_MIXERS
N_SWA_LAYERS = DEPTH // N_MIXERS
GLA_HEADS = 4
GLA_DK = D_MODEL // 2 // GLA_HEADS
GLA_DV = D_MODEL // GLA_HEADS
GLA_GATE_RANK = 16
GLA_GATE_TAU = 16.0
GLA_CHUNK = 64
GLA_IN = 2 * GLA_HEADS * GLA_DK + 2 * GLA_HEADS * GLA_DV + GLA_GATE_RANK
SWA_PATTERNS = ((128, 1), (512, 4), (2048, 16))
SWA_GROUPS = len(SWA_PATTERNS)
SWA_HEADS = 8
SWA_HEAD_DIM = D_MODEL // SWA_HEADS
SWA_IN = SWA_GROUPS * 3 * SWA_HEADS * SWA_HEAD_DIM
D_FF = 4 * D_MODEL
EPS = 1e-6

kernel_name = 'hybrid_gla_dilated_swa_sqrelu'


def _rmsnorm(x, g):
    x32 = x.astype(jnp.float32)
    y = x32 * lax.rsqrt(jnp.mean(x32 * x32, axis=-1, keepdims=True) + EPS)
    return (y * g.astype(jnp.float32)).astype(x.dtype)


def _gla_chunk_step(state, inp):
    q, k, v, g = inp
    C = q.shape[2]
    b = jnp.cumsum(g, axis=2)
    causal = jnp.tril(jnp.ones((C, C), dtype=bool))[:, :, None]
    diff = b[:, :, :, None, :] - b[:, :, None, :, :]
    decay = jnp.where(causal, jnp.exp(jnp.where(causal, diff, 0.0)), 0.0)
    attn = jnp.einsum('bhik,bhjk,bhijk->bhij', q, k, decay)
    o = (jnp.einsum('bhij,bhjv->bhiv', attn, v)
         + jnp.einsum('bhik,bhkv->bhiv', q * jnp.exp(b), state))
    b_last = b[:, :, -1, :]
    k_dec = k * jnp.exp(b_last[:, :, None, :] - b)
    new_state = state * jnp.exp(b_last)[..., None] + jnp.einsum('bhjk,bhjv->bhkv', k_dec, v)
    return new_state, o


def _gla_mixer(h, w_in, w_gate_up, b_gate, g_out, w_out):
    Bn, S, _ = h.shape
    hk = GLA_HEADS * GLA_DK
    hv = GLA_HEADS * GLA_DV
    proj = h @ w_in
    q, k, v, r, z = jnp.split(proj, [hk, 2 * hk, 2 * hk + hv, 2 * hk + 2 * hv], axis=-1)
    log_a = jax.nn.log_sigmoid((z @ w_gate_up + b_gate).astype(jnp.float32)) / GLA_GATE_TAU
    nc = S // GLA_CHUNK

    def to_chunks(t, d):
        return t.astype(jnp.float32).reshape(Bn, nc, GLA_CHUNK, GLA_HEADS, d).transpose(1, 0, 3, 2, 4)

    xs = (to_chunks(q * GLA_DK ** -0.5, GLA_DK), to_chunks(k, GLA_DK),
          to_chunks(v, GLA_DV), to_chunks(log_a, GLA_DK))
    state0 = jnp.zeros((Bn, GLA_HEADS, GLA_DK, GLA_DV), jnp.float32)
    _, o = lax.scan(_gla_chunk_step, state0, xs)
    o = o.transpose(1, 0, 3, 2, 4).reshape(Bn, S, GLA_HEADS, GLA_DV)
    o = _rmsnorm(o, g_out)
    o = o.reshape(Bn, S, hv).astype(h.dtype) * jax.nn.silu(r)
    return o @ w_out


def _dilated_band_attention(q, k, v, window, dilation):
    Bn, S, H, D = q.shape
    span = window // dilation
    blk = span
    L = S // dilation
    nb = -(-L // blk)
    Lp = nb * blk

    def to_sub(t):
        t = t.reshape(Bn, L, dilation, H, D).transpose(0, 2, 1, 3, 4).reshape(Bn * dilation, L, H, D)
        t = jnp.pad(t, ((0, 0), (0, Lp - L), (0, 0), (0, 0)))
        return t.reshape(Bn * dilation, nb, blk, H, D)

    def with_prev(t):
        prev = jnp.pad(t, ((0, 0), (1, 0), (0, 0), (0, 0), (0, 0)))[:, :-1]
        return jnp.concatenate([prev, t], axis=2)

    qs, ks, vs = to_sub(q), to_sub(k), to_sub(v)
    kk, vv = with_prev(ks), with_prev(vs)
    s = jnp.einsum('nbqhd,nbkhd->nbhqk', qs, kk).astype(jnp.float32)
    qpos = jnp.arange(nb)[:, None] * blk + jnp.arange(blk)[None, :]
    kpos = (jnp.arange(nb)[:, None] - 1) * blk + jnp.arange(2 * blk)[None, :]
    dist = qpos[:, :, None] - kpos[:, None, :]
    valid = (dist >= 0) & (dist <= span) & (kpos[:, None, :] >= 0)
    s = jnp.where(valid[None, :, None], s, -jnp.inf)
    m = jnp.max(s, axis=-1, keepdims=True)
    p = jnp.exp(s - m)
    l = jnp.sum(p, axis=-1, keepdims=True)
    o = jnp.einsum('nbhqk,nbkhd->nbhqd', p, vv.astype(jnp.float32)) / l
    lse = (m + jnp.log(l))[..., 0]
    o = o.transpose(0, 1, 3, 2, 4).reshape(Bn * dilation, Lp, H, D)[:, :L]
    o = o.reshape(Bn, dilation, L, H, D).transpose(0, 2, 1, 3, 4).reshape(Bn, S, H, D)
    lse = lse.transpose(0, 1, 3, 2).reshape(Bn * dilation, Lp, H)[:, :L]
    lse = lse.reshape(Bn, dilation, L, H).transpose(0, 2, 1, 3).reshape(Bn, S, H)
    return o, lse


def _dilated_mixer(h, w_qkv, g_q, g_k, w_out):
    Bn, S, _ = h.shape
    proj = (h @ w_qkv).reshape(Bn, S, SWA_GROUPS, 3, SWA_HEADS, SWA_HEAD_DIM)
    outs, lses = [], []
    for gi, (window, dilation) in enumerate(SWA_PATTERNS):
        q = _rmsnorm(proj[:, :, gi, 0], g_q[gi]) * SWA_HEAD_DIM ** -0.5
        k = _rmsnorm(proj[:, :, gi, 1], g_k[gi])
        o, lse = _dilated_band_attention(q, k, proj[:, :, gi, 2], window, dilation)
        outs.append(o)
        lses.append(lse)
    wts = jax.nn.softmax(jnp.stack(lses), axis=0)
    o = jnp.sum(wts[..., None] * jnp.stack(outs), axis=0)
    return o.reshape(Bn, S, SWA_HEADS * SWA_HEAD_DIM).astype(h.dtype) @ w_out


def _sqrelu_mlp(h, w_up, w_down):
    return jnp.square(jax.nn.relu(h @ w_up)) @ w_down


def _normal(key, shape, scale):
    return jax.random.normal(key, shape, jnp.float32) * scale


def setup_inputs(seed: int = 0) -> dict:
    key = jax.random.key(seed)
    ks = jax.random.split(key, 16)
    hk = GLA_HEADS * GLA_DK
    hv = GLA_HEADS * GLA_DV
    sh = SWA_HEADS * SWA_HEAD_DIM
    return {
        'x': _normal(ks[0], (BATCH, SEQ, D_MODEL), 1.0),
        'norm_mix': 1.0 + _normal(ks[1], (DEPTH, D_MODEL), 0.02),
        'norm_mlp': 1.0 + _normal(ks[2], (DEPTH, D_MODEL), 0.02),
        'gla_w_in': _normal(ks[3], (N_GLA_LAYERS, D_MODEL, GLA_IN), D_MODEL ** -0.5),
        'gla_w_gate_up': _normal(ks[4], (N_GLA_LAYERS, GLA_GATE_RANK, hk), GLA_GATE_RANK ** -0.5),
        'gla_b_gate': _normal(ks[5], (N_GLA_LAYERS, hk), 0.01),
        'gla_g_out': 1.0 + _normal(ks[6], (N_GLA_LAYERS, GLA_HEADS, GLA_DV), 0.02),
        'gla_w_out': _normal(ks[7], (N_GLA_LAYERS, hv, D_MODEL), hv ** -0.5),
        'swa_w_qkv': _normal(ks[8], (N_SWA_LAYERS, D_MODEL, SWA_IN), D_MODEL ** -0.5),
        'swa_g_q': 1.0 + _normal(ks[9], (N_SWA_LAYERS, SWA_GROUPS, SWA_HEAD_DIM), 0.02),
        'swa_g_k': 1.0 + _normal(ks[10], (N_SWA_LAYERS, SWA_GROUPS, SWA_HEAD_DIM), 0.02),
        'swa_w_out': _normal(ks[11], (N_SWA_LAYERS, sh, D_MODEL), sh ** -0.5),
        'mlp_w_up': _normal(ks[12], (DEPTH, D_MODEL, D_FF), D_MODEL ** -0.5),
        'mlp_w_down': _normal(ks[13], (DEPTH, D_FF, D_MODEL), D_FF ** -0.5),
    }


def reference(x, norm_mix, norm_mlp, gla_w_in, gla_w_gate_up, gla_b_gate, gla_g_out, gla_w_out,
              swa_w_qkv, swa_g_q, swa_g_k, swa_w_out, mlp_w_up, mlp_w_down):
    for i in range(DEPTH):
        j = i // N_MIXERS
        h = _rmsnorm(x, norm_mix[i])
        if i % N_MIXERS == 0:
            y = _gla_mixer(h, gla_w_in[j], gla_w_gate_up[j], gla_b_gate[j], gla_g_out[j], gla_w_out[j])
        else:
            y = _dilated_mixer(h, swa_w_qkv[j], swa_g_q[j], swa_g_k[j], swa_w_out[j])
        x = x + y
        x = x + _sqrelu_mlp(_rmsnorm(x, norm_mlp[i]), mlp_w_up[i], mlp_w_down[i])
    return x
```

```python
import numpy as np
import concourse.bass as bass
import concourse.mybir as mybir
from concourse.bass_utils import run_bass_kernel_spmd

F32 = mybir.dt.float32
BF16 = mybir.dt.bfloat16
AF = mybir.ActivationFunctionType
ALU = mybir.AluOpType
AX = mybir.AxisListType

SAME_ENGINE_SYNC = True


class Sched:
    ENGS = ("pe", "act", "dve", "pool", "sp")
    _semn = [0]

    def __init__(self, nc):
        self.nc = nc
        self.ops = {e: [] for e in self.ENGS}
        self.last_w = {}
        self.readers = {}
        self.chan_cnt = {}
        self.seen = {e: {} for e in self.ENGS}
        self.epoch = 0
        self.needs_inc = set()
        self.psum_keys = set()
        self.last_x = {}

    def new_epoch(self):
        self.epoch += 1

    def add(self, eng, fn, reads=(), writes=(), chan=None):
        idx = len(self.ops[eng])
        deps = []
        for k in reads:
            t = self.last_w.get(k)
            if t is not None:
                deps.append(t)
        for k in writes:
            t = self.last_w.get(k)
            if t is not None and not (chan is not None and t[0] == "d" and t[1] == chan):
                deps.append(t)
            deps.extend(self.readers.get(k, {}).values())
        xkeys = [k for k in tuple(reads) + tuple(writes) if k in self.psum_keys]
        for k in xkeys:
            t = self.last_x.get(k)
            if t is not None and t[1][0] != eng:
                deps.append(t)
        if chan is not None:
            c = self.chan_cnt.get(chan, 0) + 1
            self.chan_cnt[chan] = c
            tok = ("d", chan, c)
        else:
            tok = ("c", (eng, self.epoch), idx)
        best = {}
        for t in deps:
            kind, sk, v = t
            if kind == "c":
                if sk[0] == eng and (eng == "pe" or not SAME_ENGINE_SYNC):
                    continue
                if sk[0] == eng and v >= idx:
                    continue
            key = (kind, sk)
            if best.get(key, -1) < v:
                best[key] = v
        waits = []
        seen = self.seen[eng]
        for key, v in best.items():
            if seen.get(key, -1) >= v:
                continue
            seen[key] = v
            waits.append((key[0], key[1], v))
            if key[0] == "c":
                self.needs_inc.add((key[1], v))
        self.ops[eng].append(dict(fn=fn, waits=waits, chan=chan, epoch=self.epoch))
        for k in xkeys:
            self.last_x[k] = tok
        for k in writes:
            self.last_w[k] = tok
            self.readers[k] = {}
        for k in reads:
            self.readers.setdefault(k, {})[(tok[0], tok[1])] = tok
        return tok

    def pe(self, fn, reads=(), writes=()):
        return self.add("pe", fn, reads, writes)

    def act(self, fn, reads=(), writes=()):
        return self.add("act", fn, reads, writes)

    def dve(self, fn, reads=(), writes=()):
        return self.add("dve", fn, reads, writes)

    def pool(self, fn, reads=(), writes=()):
        return self.add("pool", fn, reads, writes)

    def dma(self, queue, chan, out, in_, reads=(), writes=()):
        return self.add(queue, lambda e: e.dma_start(out=out, in_=in_), reads, writes, chan=chan)

    def emit(self, final_chans=()):
        nc = self.nc
        last_c = {}
        for e in self.ENGS:
            for idx in range(len(self.ops[e]) - 1, -1, -1):
                if self.ops[e][idx]["chan"] is None:
                    sk = (e, self.ops[e][idx]["epoch"])
                    self.needs_inc.add((sk, idx))
                    last_c[e] = (sk, idx)
                    break
        cnt = {}
        incval = {}
        for e in self.ENGS:
            for idx, op in enumerate(self.ops[e]):
                sk = (e, op["epoch"])
                if (sk, idx) in self.needs_inc:
                    cnt[sk] = cnt.get(sk, 0) + 1
                    incval[(sk, idx)] = cnt[sk]
        sem_keys = sorted(cnt.keys()) + sorted(("chan", c) for c in self.chan_cnt)
        sems = {}
        for k in sem_keys:
            Sched._semn[0] += 1
            sems[k] = nc.alloc_semaphore("sem%d" % Sched._semn[0])
        with nc.Block() as block:

            def run(ename, eng):
                for idx, op in enumerate(self.ops[ename]):
                    for kind, sk, v in op["waits"]:
                        if kind == "c":
                            eng.wait_ge(sems[sk], incval[(sk, v)])
                        else:
                            eng.wait_ge(sems[("chan", sk)], 16 * v)
                    ins = op["fn"](eng)
                    sk = (ename, op["epoch"])
                    if op["chan"] is not None:
                        ins.then_inc(sems[("chan", op["chan"])], 16)
                    elif (sk, idx) in incval:
                        ins.then_inc(sems[sk], 1)
                for e2, (sk, idx) in last_c.items():
                    if e2 != ename:
                        eng.wait_ge(sems[sk], incval[(sk, idx)])
                for c, n in self.chan_cnt.items():
                    eng.wait_ge(sems[("chan", c)], 16 * n)

            @block.tensor
            def _(e):
                run("pe", e)

            @block.scalar
            def _(e):
                run("act", e)

            @block.vector
            def _(e):
                run("dve", e)

            @block.gpsimd
            def _(e):
                run("pool", e)

            @block.sync
            def _(e):
                run("sp", e)
        nc.all_engine_barrier()
        nc.clear_and_free_semaphores(list(sems.values()))
        nc.all_engine_barrier()

    def barrier_deps(self):
        return None


D = 1024
DFF = 4096
EPS = 1e-6
NT = 128


class Ctx:
    _n = [0]

    def __init__(self, nc, es):
        self.nc = nc
        self.es = es
        self.S = Sched(nc)
        Ctx._n[0] += 1
        self.tag = "p%d_" % Ctx._n[0]

    def sb(self, name, shape, dt):
        return self.es.enter_context(self.nc.sbuf_tensor(self.tag + name, shape, dt))

    def ps(self, name, shape, dt):
        return self.es.enter_context(self.nc.psum_tensor(self.tag + name, shape, dt))


def emit_norm_tile(C, xin, xkey, hb, hbkey, junk, st, stkey):
    S = C.S
    S.pool(lambda e: e.memset(st[:, 0:1], 0.0), writes=[stkey])
    S.act(lambda e: e.activation(out=junk[:], in_=xin[:], func=AF.Square, scale=1.0 / 32.0,
                                 accum_out=st[:, 0:1]), reads=[xkey, stkey], writes=["junk", stkey])
    S.act(lambda e: e.activation(out=st[:, 1:2], in_=st[:, 0:1], func=AF.Sqrt, bias=EPS, scale=1.0),
          reads=[stkey], writes=[stkey])
    S.dve(lambda e: e.reciprocal(out=st[:, 2:3], in_=st[:, 1:2]), reads=[stkey], writes=[stkey])
    S.act(lambda e: e.activation(out=hb[:], in_=xin[:], func=AF.Copy, scale=st[:, 2:3]),
          reads=[xkey, stkey], writes=[hbkey])


def emit_transpose_tile(C, hb, hbkey, idb, ptr, ptrkey, hT_dst, hTkey, gcol):
    S = C.S
    for c in range(8):
        S.pe(lambda e, c=c: e.transpose(out=ptr[:, c, :], in_=hb[:, c * 128:(c + 1) * 128], identity=idb[:]),
             reads=[hbkey, "idb"], writes=[ptrkey])
    S.dve(lambda e: e.tensor_tensor(out=hT_dst, in0=ptr[:, :, :],
                                    in1=gcol[:, :].unsqueeze(2).to_broadcast([128, 8, 128]), op=ALU.mult),
          reads=[ptrkey, "gcol"], writes=[hTkey])


def phase_mlp(nc, x_src, x_dst, g_ap, wup, wdn, ident, S_len):
    import contextlib
    M = S_len // 512
    with contextlib.ExitStack() as es:
        C = Ctx(nc, es)
        S = C.S
        wup_sb = C.sb("wup", [128, 8, DFF], BF16)
        wdn_sb = C.sb("wdn", [128, 32, D], BF16)
        idb = C.sb("idb", [128, 128], BF16)
        gcol = C.sb("gcol", [128, 8], F32)
        xn = [C.sb("xn%d" % i, [128, D], F32) for i in range(2)]
        hb = [C.sb("hb%d" % i, [128, D], BF16) for i in range(4)]
        st = [C.sb("st%d" % i, [128, 4], F32) for i in range(2)]
        junk = C.sb("junk", [128, D], BF16)
        hT = [C.sb("hT%d" % i, [128, 8, 512], BF16) for i in range(2)]
        actT = C.sb("actT", [128, 32, 512], BF16)
        rl = [C.sb("rl%d" % i, [128, 512], F32) for i in range(2)]
        xr = [C.sb("xr%d" % i, [128, D], F32) for i in range(2)]
        pu = [C.ps("pu%d" % i, [128, 512], F32) for i in range(3)]
        pd = [C.ps("pd%d" % i, [128, 512], F32) for i in range(4)]
        ptr = C.ps("ptr", [128, 8, 128], BF16)
        S.psum_keys.update([("pu", i) for i in range(3)] + [("pd", i) for i in range(4)] + ["ptr"])

        S.dma("pool", "c_id", idb[:], ident, writes=["idb"])
        S.dma("sp", "c_g", gcol[:], g_ap, writes=["gcol"])
        wup_v = wup.rearrange("(kc p) n -> p kc n", p=128)
        wdn_v = wdn.rearrange("(fc p) n -> p fc n", p=128)
        for g in range(8):
            S.dma("pool", "w_up%d" % g, wup_sb[:, :, g * 512:(g + 1) * 512], wup_v[:, :, g * 512:(g + 1) * 512],
                  writes=[("wup", g)])
        for g in range(8):
            S.dma("pool", "w_dn%d" % g, wdn_sb[:, g * 4:(g + 1) * 4, :], wdn_v[:, g * 4:(g + 1) * 4, :],
                  writes=[("wdn", g)])

        def load_xn(j):
            s = j % 2
            S.dma("sp", "xn%d" % s, xn[s][:], x_src[j * 128:(j + 1) * 128, :], writes=[("xn", s)])

        def norm_elem(j):
            s = j % 2
            emit_norm_tile(C, xn[s], ("xn", s), hb[j % 4], ("hb", j % 4), junk, st[s], ("st", s))

        def transp(j):
            m, q = divmod(j, 4)
            emit_transpose_tile(C, hb[q], ("hb", q), idb, ptr, "ptr",
                                hT[m % 2][:, :, q * 128:(q + 1) * 128], ("hT", m % 2), gcol)

        for q in range(4):
            load_xn(q)
            norm_elem(q)
            transp(q)
        for m in range(M):
            ms = m % 2
            for g in range(32):
                bank = pu[g % 3]
                for kc in range(8):
                    S.pe(lambda e, g=g, kc=kc, bank=bank, ms=ms: e.matmul(
                        bank[:], lhsT=wup_sb[:, kc, g * 128:(g + 1) * 128], rhs=hT[ms][:, kc, :],
                        start=(kc == 0), stop=(kc == 7)),
                        reads=[("wup", g // 4), ("hT", ms)], writes=[("pu", g % 3)])
                r = rl[g % 2]
                S.act(lambda e, bank=bank, r=r: e.activation(out=r[:], in_=bank[:], func=AF.Relu),
                      reads=[("pu", g % 3)], writes=[("rl", g % 2)])
                S.dve(lambda e, g=g, r=r: e.tensor_tensor(out=actT[:, g, :], in0=r[:], in1=r[:], op=ALU.mult),
                      reads=[("rl", g % 2)], writes=["actT"])
                if m + 1 < M and g % 8 == 1:
                    q = g // 8
                    load_xn((m + 1) * 4 + q)
                    norm_elem((m + 1) * 4 + q)
            if m + 1 < M:
                for q in range(4):
                    transp((m + 1) * 4 + q)
            for q in range(4):
                j = m * 4 + q
                s = j % 2
                S.dma("pool", "xr%d" % s, xr[s][:], x_src[j * 128:(j + 1) * 128, :], writes=[("xr", s)])
                for half in range(2):
                    bank = pd[(2 * q + half) % 4]
                    bk = ("pd", (2 * q + half) % 4)
                    for fc in range(32):
                        S.pe(lambda e, fc=fc, q=q, half=half, bank=bank: e.matmul(
                            bank[:], lhsT=actT[:, fc, q * 128:(q + 1) * 128],
                            rhs=wdn_sb[:, fc, half * 512:(half + 1) * 512],
                            start=(fc == 0), stop=(fc == 31)),
                            reads=["actT", ("wdn", fc // 4)], writes=[bk])
                    S.dve(lambda e, s=s, half=half, bank=bank: e.tensor_tensor(
                        out=xr[s][:, half * 512:(half + 1) * 512], in0=bank[:],
                        in1=xr[s][:, half * 512:(half + 1) * 512], op=ALU.add),
                        reads=[bk, ("xr", s)], writes=[("xr", s)])
                S.dma("pool", "xs%d" % s, x_dst[j * 128:(j + 1) * 128, :], xr[s][:], reads=[("xr", s)])
        S.emit(final_chans=["xs0", "xs1"])


GLA_H = 4
STAGE = 99
GLA_IN = 3088


def emit_rstd(C, st, stkey, n):
    S = C.S
    S.act(lambda e: e.activation(out=st[:, n:2 * n], in_=st[:, 0:n], func=AF.Ln, bias=EPS, scale=1.0),
          reads=[stkey], writes=[stkey])
    S.act(lambda e: e.activation(out=st[:, 2 * n:3 * n], in_=st[:, n:2 * n], func=AF.Exp, scale=-0.5),
          reads=[stkey], writes=[stkey])


def emit_norm_tile2(C, xin, xkey, hb, hbkey, junk, st, stkey):
    S = C.S
    S.pool(lambda e: e.memset(st[:, 0:1], 0.0), writes=[stkey])
    S.act(lambda e: e.activation(out=junk[:], in_=xin[:], func=AF.Square, scale=1.0 / 32.0,
                                 accum_out=st[:, 0:1]), reads=[xkey, stkey], writes=["junk", stkey])
    emit_rstd(C, st, stkey, 1)
    S.act(lambda e: e.activation(out=hb[:], in_=xin[:], func=AF.Copy, scale=st[:, 2:3]),
          reads=[xkey, stkey], writes=[hbkey])


def phase_gla(nc, x_src, x_dst, g_ap, w_in, w_gate, b_gate, g_out, w_out, ident, tri, trineg, S_len):
    import contextlib
    M = S_len // 512
    with contextlib.ExitStack() as es:
        C = Ctx(nc, es)
        S = C.S
        win_sb = C.sb("win", [128, 8, GLA_IN], BF16)
        wout_sb = C.sb("wout", [128, 8, D], BF16)
        idb = C.sb("idb", [128, 128], BF16)
        gcol = C.sb("gcol", [128, 8], F32)
        trim = C.sb("trim", [128, 128], F32)
        trin = C.sb("trin", [128, 128], BF16)
        wg_sb = C.sb("wg", [128, 512], F32)
        wg_hi = C.sb("wg_hi", [128, 512], BF16)
        wg_lo = C.sb("wg_lo", [128, 512], BF16)
        zT_hi = C.sb("zT_hi", [128, 512], BF16)
        zT_lo = C.sb("zT_lo", [128, 512], BF16)
        goutB = C.sb("goutB", [128, D], F32)
        xn = [C.sb("xn%d" % i, [128, D], F32) for i in range(2)]
        hb = [C.sb("hb0", [128, D], BF16)]
        st = [C.sb("st%d" % i, [128, 4], F32) for i in range(2)]
        junk = C.sb("junk", [128, D], BF16)
        hT = C.sb("hT", [128, 8, 512], BF16)
        qTf = C.sb("qTf", [128, 4, 512], F32)
        kTf = C.sb("kTf", [128, 4, 512], F32)
        zT = C.sb("zT", [128, 512], F32)
        v_sb = C.sb("v", [128, 4, D], BF16)
        r_sb = C.sb("r", [128, D], F32)
        sr = C.sb("sr", [128, 4, D], F32)
        et = C.sb("et", [128, D], F32)
        glf = C.sb("glf", [128, 512], F32)
        gl_hi = C.sb("gl_hi", [128, 4, 512], BF16)
        gl_lo = C.sb("gl_lo", [128, 4, 512], BF16)
        eb = [C.sb("eb%d" % i, [128, 4, 128], F32) for i in range(2)]
        enb = C.sb("enb", [128, 4, 128], F32)
        qt = [C.sb("qt%d" % i, [128, 4, 128], BF16) for i in range(2)]
        kt = [C.sb("kt%d" % i, [128, 4, 128], BF16) for i in range(2)]
        ktok = C.sb("ktok", [128, 4, 128], BF16)
        AT = C.sb("AT", [128, 4, 128], BF16)
        St = C.sb("St", [128, 4, 256], F32)
        Sbf = [C.sb("Sbf%d" % i, [128, 4, 256], BF16) for i in range(2)]
        ost = C.sb("ost", [128, 12], F32)
        on = C.sb("on", [128, 4, 256], F32)
        gated = C.sb("gated", [128, D], BF16)
        gT = C.sb("gT", [128, 8, 128], BF16)
        xr = [C.sb("xr%d" % i, [128, D], F32) for i in range(2)]
        pp = [C.ps("pp%d" % i, [128, 512], F32) for i in range(2)]
        ptr = C.ps("ptr", [128, 8, 128], BF16)
        pba = C.ps("pba", [128, 4, 128], F32)
        po = C.ps("po", [128, 4, 256], F32)
        pS = C.ps("pS", [128, 4, 256], F32)
        S.psum_keys.update([("pp", 0), ("pp", 1), "ptr", "pba", "po", "pS"])

        S.dma("pool", "c_id", idb[:], ident, writes=["idb"])
        S.dma("sp", "c_g", gcol[:], g_ap, writes=["gcol"])
        S.dma("sp", "c_t1", trim[:], tri, writes=["trim"])
        S.dma("pool", "c_t2", trin[:], trineg, writes=["trin"])
        S.pool(lambda e: e.memset(wg_sb[:], 0.0), writes=["wg"])
        S.dma("sp", "c_wg", wg_sb[0:16, :], w_gate, writes=["wg"])
        S.dma("sp", "c_wg", wg_sb[16:17, :], b_gate, writes=["wg"])
        S.dve(lambda e: e.tensor_copy(out=wg_hi[:], in_=wg_sb[:]), reads=["wg"], writes=["wg_hi"])
        S.dve(lambda e: e.tensor_tensor(out=wg_lo[:], in0=wg_sb[:], in1=wg_hi[:], op=ALU.subtract),
              reads=["wg", "wg_hi"], writes=["wg_lo"])
        S.pool(lambda e: e.memset(zT[:], 0.0), writes=["zT"])
        S.pool(lambda e: e.memset(zT[0:32, :], 1.0), writes=["zT"])
        S.dma("sp", "c_go", goutB[:], g_out.partition_broadcast(128), writes=["goutB"])
        S.pool(lambda e: e.memset(St[:], 0.0), writes=["St"])
        S.pool(lambda e: e.memset(Sbf[0][:], 0.0), writes=[("Sbf", 0)])
        win_v = w_in.rearrange("(kc p) n -> p kc n", p=128)
        wout_v = w_out.rearrange("(kc p) n -> p kc n", p=128)
        blocks = [(0, 1024), (1024, 2048), (2048, 3072), (3072, 3088)]
        for bi, (c0, c1) in enumerate(blocks):
            S.dma("pool", "w_in%d" % bi, win_sb[:, :, c0:c1], win_v[:, :, c0:c1], writes=[("win", bi)])
        S.dma("pool", "w_out", wout_sb[:], wout_v, writes=["wout"])

        pcnt = [0]

        def pbank():
            i = pcnt[0] % 2
            pcnt[0] += 1
            return pp[i], ("pp", i)

        def P0(m):
            for q in range(4):
                j = m * 4 + q
                s = j % 2
                S.dma("sp", "xn%d" % s, xn[s][:], x_src[j * 128:(j + 1) * 128, :], writes=[("xn", s)])
                emit_norm_tile2(C, xn[s], ("xn", s), hb[0], ("hb", 0), junk, st[s], ("st", s))
                emit_transpose_tile(C, hb[0], ("hb", 0), idb, ptr, "ptr",
                                    hT[:, :, q * 128:(q + 1) * 128], "hT", gcol)
            bank, bk = pbank()
            for kc in range(8):
                S.pe(lambda e, kc=kc, bank=bank: e.matmul(
                    bank[0:16, :], lhsT=win_sb[:, kc, 3072:3088], rhs=hT[:, kc, :],
                    start=(kc == 0), stop=(kc == 7)), reads=[("win", 3), "hT"], writes=[bk])
            S.dve(lambda e, bank=bank: e.tensor_copy(out=zT[0:16, :], in_=bank[0:16, :]), reads=[bk], writes=["zT"])
            S.dve(lambda e: e.tensor_copy(out=zT_hi[:], in_=zT[:]), reads=["zT"], writes=["zT_hi"])
            S.dve(lambda e: e.tensor_tensor(out=zT_lo[:], in0=zT[:], in1=zT_hi[:], op=ALU.subtract),
                  reads=["zT", "zT_hi"], writes=["zT_lo"])

        def Pqk(m):
            for gi in range(8):
                bank, bk = pbank()
                for kc in range(8):
                    S.pe(lambda e, gi=gi, kc=kc, bank=bank: e.matmul(
                        bank[:], lhsT=win_sb[:, kc, gi * 128:(gi + 1) * 128], rhs=hT[:, kc, :],
                        start=(kc == 0), stop=(kc == 7)), reads=[("win", 0), "hT"], writes=[bk])
                if gi < 4:
                    S.act(lambda e, gi=gi, bank=bank: e.activation(out=qTf[:, gi, :], in_=bank[:], func=AF.Copy,
                                                                   scale=128.0 ** -0.5),
                          reads=[bk], writes=["qTf"])
                else:
                    S.dve(lambda e, gi=gi, bank=bank: e.tensor_copy(out=kTf[:, gi - 4, :], in_=bank[:]),
                          reads=[bk], writes=["kTf"])

        def Pv(m, q):
            for half in range(2):
                bank, bk = pbank()
                for kc in range(8):
                    S.pe(lambda e, q=q, half=half, kc=kc, bank=bank: e.matmul(
                        bank[:], lhsT=hT[:, kc, q * 128:(q + 1) * 128],
                        rhs=win_sb[:, kc, 1024 + half * 512:1024 + (half + 1) * 512],
                        start=(kc == 0), stop=(kc == 7)), reads=[("win", 1), "hT"], writes=[bk])
                S.dve(lambda e, q=q, half=half, bank=bank: e.tensor_copy(
                    out=v_sb[:, q, half * 512:(half + 1) * 512], in_=bank[:]), reads=[bk], writes=[("v", q)])
            for half in range(2):
                bank, bk = pbank()
                for kc in range(8):
                    S.pe(lambda e, q=q, half=half, kc=kc, bank=bank: e.matmul(
                        bank[:], lhsT=hT[:, kc, q * 128:(q + 1) * 128],
                        rhs=win_sb[:, kc, 2048 + half * 512:2048 + (half + 1) * 512],
                        start=(kc == 0), stop=(kc == 7)), reads=[("win", 2), "hT"], writes=[bk])
                hs = slice(half * 512, (half + 1) * 512)
                S.act(lambda e, hs=hs, bank=bank: e.activation(out=et[:, hs], in_=bank[:], func=AF.Exp, scale=-1.0),
                      reads=[bk], writes=["et"])
                S.dve(lambda e, hs=hs, bank=bank: e.tensor_copy(out=r_sb[:, hs], in_=bank[:]),
                      reads=[bk], writes=["r"])
            S.act(lambda e: e.activation(out=et[:], in_=et[:], func=AF.Ln, bias=1.0, scale=1.0),
                  reads=["et"], writes=["et"])
            S.act(lambda e, q=q: e.activation(out=sr[:, q, :], in_=et[:], func=AF.Exp, scale=-1.0),
                  reads=["et"], writes=[("sr", q)])
            S.pool(lambda e, q=q: e.tensor_tensor(out=sr[:, q, :], in0=sr[:, q, :], in1=r_sb[:, :], op=ALU.mult),
                   reads=[("sr", q), "r"], writes=[("sr", q)])
            S.pool(lambda e, q=q: e.tensor_tensor(out=sr[:, q, :], in0=sr[:, q, :], in1=goutB[:], op=ALU.mult),
                   reads=[("sr", q), "goutB"], writes=[("sr", q)])
            bank, bk = pbank()
            qs = slice(q * 128, (q + 1) * 128)
            for pi, (za, wa) in enumerate(((zT_hi, wg_hi), (zT_lo, wg_hi), (zT_hi, wg_lo))):
                S.pe(lambda e, qs=qs, bank=bank, za=za, wa=wa, pi=pi: e.matmul(
                    bank[:], lhsT=za[:, qs], rhs=wa[:, :], start=(pi == 0), stop=(pi == 2)),
                    reads=["zT_hi", "zT_lo", "wg_hi", "wg_lo"], writes=[bk])
            S.act(lambda e, bank=bank: e.activation(out=glf[:], in_=bank[:], func=AF.Exp, scale=-1.0),
                  reads=[bk], writes=["glf"])
            S.act(lambda e: e.activation(out=glf[:], in_=glf[:], func=AF.Ln, bias=1.0, scale=1.0),
                  reads=["glf"], writes=["glf"])
            S.dve(lambda e, q=q: e.tensor_copy(out=gl_hi[:, q, :], in_=glf[:]), reads=["glf"], writes=[("gl", q)])
            S.dve(lambda e, q=q: e.tensor_tensor(out=gl_lo[:, q, :], in0=glf[:], in1=gl_hi[:, q, :], op=ALU.subtract),
                  reads=["glf", ("gl", q)], writes=[("gl", q)])

        def Aa(m, q):
            e2 = q % 2
            ts = slice(q * 128, (q + 1) * 128)
            for h in range(4):
                for pi, ga in enumerate((gl_hi, gl_lo)):
                    S.pe(lambda e, q=q, h=h, ga=ga, pi=pi: e.matmul(
                        pba[:, h, :], lhsT=ga[:, q, h * 128:(h + 1) * 128], rhs=trin[:, :],
                        start=(pi == 0), stop=(pi == 1)), reads=[("gl", q), "trin"], writes=["pba"])
            S.act(lambda e, e2=e2: e.activation(out=eb[e2][:], in_=pba[:], func=AF.Exp), reads=["pba"], writes=[("eb", e2)])
            S.act(lambda e: e.activation(out=enb[:], in_=pba[:], func=AF.Exp, scale=-1.0), reads=["pba"], writes=["enb"])
            S.dve(lambda e, ts=ts, e2=e2: e.tensor_tensor(out=qt[e2][:], in0=qTf[:, :, ts], in1=eb[e2][:], op=ALU.mult),
                  reads=["qTf", ("eb", e2)], writes=[("qt", e2)])
            S.dve(lambda e, ts=ts, e2=e2: e.tensor_tensor(out=kt[e2][:], in0=kTf[:, :, ts], in1=enb[:], op=ALU.mult),
                  reads=["kTf", "enb"], writes=[("kt", e2)])

        def Ab(m, q):
            e2 = q % 2
            for h in range(4):
                S.pe(lambda e, h=h, e2=e2: e.transpose(out=ptr[:, h, :], in_=kt[e2][:, h, :], identity=idb[:]),
                     reads=[("kt", e2), "idb"], writes=["ptr"])
            S.act(lambda e: e.copy(out=ktok[:], in_=ptr[:, 0:4, :]), reads=["ptr"], writes=["ktok"])
            for h in range(4):
                S.pe(lambda e, h=h, e2=e2: e.matmul(pba[:, h, :], lhsT=kt[e2][:, h, :], rhs=qt[e2][:, h, :],
                                                    start=True, stop=True),
                     reads=[("kt", e2), ("qt", e2)], writes=["pba"])
            S.dve(lambda e: e.tensor_tensor(out=AT[:], in0=pba[:],
                                            in1=trim[:, :].unsqueeze(1).to_broadcast([128, 4, 128]), op=ALU.mult),
                  reads=["pba", "trim"], writes=["AT"])

        def Bs(m, q):
            e2 = q % 2
            c = m * 4 + q
            sb_in = Sbf[c % 2]
            so = Sbf[(c + 1) % 2]
            for h in range(4):
                S.pe(lambda e, q=q, h=h: e.matmul(po[:, h, :], lhsT=AT[:, h, :], rhs=v_sb[:, q, h * 256:(h + 1) * 256],
                                                  start=(h % 2 == 0), stop=False, skip_group_check=True),
                     reads=["AT", ("v", q)], writes=["po"])
            for h in range(4):
                S.pe(lambda e, h=h, e2=e2, sb_in=sb_in: e.matmul(
                    po[:, h, :], lhsT=qt[e2][:, h, :], rhs=sb_in[:, h, :], start=False, stop=True,
                    skip_group_check=True), reads=[("qt", e2), ("Sbf", c % 2)], writes=["po"])
            for h in range(4):
                S.pe(lambda e, q=q, h=h: e.matmul(
                    pS[:, h, :], lhsT=ktok[:, h, :], rhs=v_sb[:, q, h * 256:(h + 1) * 256],
                    start=True, stop=True), reads=["ktok", ("v", q)], writes=["pS"])
            S.dve(lambda e: e.tensor_tensor(out=St[:], in0=pS[:], in1=St[:], op=ALU.add),
                  reads=["pS", "St"], writes=["St"])
            S.dve(lambda e, e2=e2: e.tensor_tensor(
                out=St[:], in0=St[:], in1=eb[e2][:, :, 127:128].to_broadcast([128, 4, 256]), op=ALU.mult),
                reads=["St", ("eb", e2)], writes=["St"])
            S.act(lambda e, so=so: e.copy(out=so[:], in_=St[:]), reads=["St"], writes=[("Sbf", (c + 1) % 2)])

        def Ca(m, q):
            S.pool(lambda e: e.memset(ost[:, 0:4], 0.0), writes=["ost"])
            for h in range(4):
                S.act(lambda e, h=h: e.activation(out=junk[:, 0:256], in_=po[:, h, :], func=AF.Square, scale=1.0 / 16.0,
                                                  accum_out=ost[:, h:h + 1]), reads=["po", "ost"], writes=["junk", "ost"])
            emit_rstd(C, ost, "ost", 4)
            S.dve(lambda e: e.tensor_tensor(out=on[:], in0=po[:], in1=ost[:, 8:12].unsqueeze(2).to_broadcast([128, 4, 256]),
                                            op=ALU.mult), reads=["po", "ost"], writes=["on"])
            S.pool(lambda e, q=q: e.tensor_tensor(out=gated[:], in0=on[:].rearrange("p h v -> p (h v)"), in1=sr[:, q, :],
                                                  op=ALU.mult), reads=["on", ("sr", q)], writes=["gated"])

        def Cb(m, q):
            for c8 in range(8):
                S.pe(lambda e, c8=c8: e.transpose(out=ptr[:, c8, :], in_=gated[:, c8 * 128:(c8 + 1) * 128], identity=idb[:]),
                     reads=["gated", "idb"], writes=["ptr"])
            S.act(lambda e: e.copy(out=gT[:], in_=ptr[:]), reads=["ptr"], writes=["gT"])

        def Cc(m, q):
            j = m * 4 + q
            s = j % 2
            S.dma("pool", "xr%d" % s, xr[s][:], x_src[j * 128:(j + 1) * 128, :], writes=[("xr", s)])
            for half in range(2):
                bank, bk = pbank()
                for c8 in range(8):
                    S.pe(lambda e, c8=c8, half=half, bank=bank: e.matmul(
                        bank[:], lhsT=gT[:, c8, :], rhs=wout_sb[:, c8, half * 512:(half + 1) * 512],
                        start=(c8 == 0), stop=(c8 == 7)), reads=["gT", "wout"], writes=[bk])
                S.dve(lambda e, s=s, half=half, bank=bank: e.tensor_tensor(
                    out=xr[s][:, half * 512:(half + 1) * 512], in0=bank[:],
                    in1=xr[s][:, half * 512:(half + 1) * 512], op=ALU.add),
                    reads=[bk, ("xr", s)], writes=[("xr", s)])
            S.dma("pool", "xs%d" % s, x_dst[j * 128:(j + 1) * 128, :], xr[s][:], reads=[("xr", s)])


        P0(0)
        Pqk(0)
        for q in range(4):
            Pv(0, q)
        for m in range(M):
            nxt = m + 1 < M
            for n in range(4 + 3):
                if 0 <= n - 1 < 4:
                    Ab(m, n - 1)
                if 0 <= n - 3 < 4:
                    Cc(m, n - 3)
                if n < 4:
                    Aa(m, n)
                if 0 <= n - 2 < 4:
                    Cb(m, n - 2)
                if 0 <= n - 1 < 4:
                    Bs(m, n - 1)
                    Ca(m, n - 1)
                if nxt:
                    if n == 1:
                        P0(m + 1)
                    if n == 3:
                        Pqk(m + 1)
                    if 2 <= n <= 5:
                        Pv(m + 1, n - 2)

        S.emit(final_chans=["xs0", "xs1"])


SWA_PATTERNS = ((128, 1), (512, 4), (2048, 16))
SWA_H = 8
SWA_IN = 9216


def phase_swa_attn(nc, x_src, onT, g_ap, w_qkv, gq_ap, gk_ap, ident, maskneg, S_len):
    import contextlib
    import math
    NTL = S_len // 128
    NCH = S_len // 512
    with contextlib.ExitStack() as es:
        C = Ctx(nc, es)
        S = C.S
        hT = C.sb("hT", [128, 8, S_len], BF16)
        idb = C.sb("idb", [128, 128], BF16)
        onesb = C.sb("onesb", [128, 128], BF16)
        mneg = C.sb("mneg", [128, 256], BF16)
        gcol = C.sb("gcol", [128, 8], F32)
        gq = C.sb("gq", [128, 3], F32)
        gk = C.sb("gk", [128, 3], F32)
        xn = [C.sb("xn%d" % i, [128, D], F32) for i in range(2)]
        hb = [C.sb("hb%d" % i, [128, D], BF16) for i in range(2)]
        st = [C.sb("st%d" % i, [128, 4], F32) for i in range(2)]
        junk = C.sb("junk", [128, D], BF16)
        wq = [C.sb("wq%d" % i, [128, 8, 128], BF16) for i in range(2)]
        wk = [C.sb("wk%d" % i, [128, 8, 128], BF16) for i in range(2)]
        wv = [C.sb("wv%d" % i, [128, 8, 128], BF16) for i in range(2)]
        qT = C.sb("qT", [128, S_len], BF16)
        kT = C.sb("kT", [128, S_len], BF16)
        V = C.sb("V", [128, NTL, 128], BF16)
        raw = [C.sb("raw%d" % i, [128, 512], F32) for i in range(2)]
        sq = [C.sb("sq%d" % i, [128, 512], BF16) for i in range(2)]
        rs = [C.sb("rs%d" % i, [128, 512], F32) for i in range(2)]
        PT = [C.sb("PT%d" % i, [128, 256], BF16) for i in range(3)]
        oacc = C.sb("oacc", [128, S_len], F32)
        lacc = C.sb("lacc", [128, S_len], F32)
        onb = C.sb("onb", [128, S_len], BF16)
        pq = [C.ps("pq%d" % i, [128, 512], F32) for i in range(2)]
        pst = pq
        pss = C.ps("pss", [128, 512], F32)
        pvf = C.ps("pvf", [128, 4, 128], F32)
        pv = pvf[:].bitcast(BF16).rearrange("p a (b c) -> p (a b) c", c=128)
        po = [C.ps("po%d" % i, [128, 4, 128], F32) for i in range(2)]
        pl = [C.ps("pl%d" % i, [128, 4, 128], F32) for i in range(2)]
        S.psum_keys.update([("pq", 0), ("pq", 1), "pss", "pvf", ("po", 0), ("po", 1), ("pl", 0), ("pl", 1)])

        S.dma("pool", "c_id", idb[:], ident, writes=["idb"])
        S.dma("pool", "c_mn", mneg[:], maskneg, writes=["mneg"])
        S.dma("sp", "c_g", gcol[:], g_ap, writes=["gcol"])
        S.dma("sp", "c_gq", gq[:], gq_ap, writes=["gq"])
        S.dma("sp", "c_gk", gk[:], gk_ap, writes=["gk"])
        S.pool(lambda e: e.memset(onesb[:], 1.0), writes=["onesb"])
        wv_all = w_qkv.rearrange("(kc p) n -> p kc n", p=128)

        units = [(h, gi) for h in range(SWA_H) for gi in range(3)]

        def load_w(u):
            h, gi = units[u]
            s = u % 2
            for t, wt, nm in ((0, wq, "wq"), (1, wk, "wk"), (2, wv, "wv")):
                c0 = ((gi * 3 + t) * SWA_H + h) * 128
                S.dma("pool", "%s%d" % (nm, s), wt[s][:], wv_all[:, :, c0:c0 + 128], writes=[(nm, s)])

        load_w(0)
        for j in range(NTL):
            s = j % 2
            S.dma("sp", "xn%d" % s, xn[s][:], x_src[j * 128:(j + 1) * 128, :], writes=[("xn", s)])
            emit_norm_tile2(C, xn[s], ("xn", s), hb[s], ("hb", s), junk, st[s], ("st", s))
            emit_transpose_tile(C, hb[s], ("hb", s), idb, pv, "pvf", hT[:, :, j * 128:(j + 1) * 128], ("hT", j // 4), gcol)

        LN_SCALE = -0.5 * math.log(128.0)
        for u, (h, gi) in enumerate(units):
            s = u % 2
            if u + 1 < len(units):
                load_w(u + 1)
            window, Dg = SWA_PATTERNS[gi]
            L = S_len // Dg
            nb = L // 128
            def proj_mm(ci, wt, nm, ch):
                cs = slice(ch * 512, (ch + 1) * 512)
                i2 = ci % 2
                bank = pq[i2]
                for kc in range(8):
                    S.pe(lambda e, kc=kc, bank=bank, wt=wt, s=s, cs=cs: e.matmul(
                        bank[:], lhsT=wt[s][:, kc, :], rhs=hT[:, kc, cs], start=(kc == 0), stop=(kc == 7)),
                        reads=[(nm, s), ("hT", ch)], writes=[("pq", i2)])
                S.act(lambda e, bank=bank, i2=i2: e.activation(out=sq[i2][:], in_=bank[:], func=AF.Square),
                      reads=[("pq", i2)], writes=[("sq", i2)])
                S.dve(lambda e, bank=bank, i2=i2: e.tensor_copy(out=raw[i2][:], in_=bank[:]),
                      reads=[("pq", i2)], writes=[("raw", i2)])

            def proj_norm(ci, dst, dkey, gain, extra, ch):
                cs = slice(ch * 512, (ch + 1) * 512)
                i2 = ci % 2
                S.pe(lambda e, i2=i2: e.matmul(pss[:], lhsT=onesb[:], rhs=sq[i2][:], start=True, stop=True),
                     reads=["onesb", ("sq", i2)], writes=["pss"])
                S.act(lambda e, i2=i2: e.activation(out=rs[i2][:], in_=pss[:], func=AF.Ln, bias=EPS, scale=1.0 / 128.0),
                      reads=["pss"], writes=[("rs", i2)])
                S.act(lambda e, i2=i2, extra=extra: e.activation(out=rs[i2][:], in_=rs[i2][:], func=AF.Exp, scale=-0.5,
                                                                 bias=extra),
                      reads=[("rs", i2)], writes=[("rs", i2)])
                S.dve(lambda e, i2=i2, dst=dst, cs=cs, gain=gain, gi=gi: e.scalar_tensor_tensor(
                    out=dst[:, cs], in0=raw[i2][:], scalar=gain[:, gi:gi + 1], in1=rs[i2][:],
                    op0=ALU.mult, op1=ALU.mult), reads=[("raw", i2), ("rs", i2), "gq", "gk"], writes=[dkey])

            def v_group(b4):
                for bb in range(4):
                    blk = b4 * 4 + bb
                    r, b = divmod(blk, nb)
                    t0 = Dg * 128 * b + r
                    for kc in range(8):
                        S.pe(lambda e, kc=kc, bb=bb, t0=t0, Dg=Dg, s=s: e.matmul(
                            pvf[:, bb, :], lhsT=hT[:, kc, t0:t0 + 127 * Dg + 1:Dg], rhs=wv[s][:, kc, :],
                            start=(kc == 0), stop=(kc == 7)),
                            reads=[("hT", c_) for c_ in range(t0 // 512, (t0 + 127 * Dg) // 512 + 1)] + [("wv", s)],
                            writes=["pvf"])
                S.act(lambda e, b4=b4: e.copy(out=V[:, b4 * 4:(b4 + 1) * 4, :], in_=pvf[:]),
                      reads=["pvf"], writes=["V"])

            plist = [(wq, "wq", qT, "qT", gq, LN_SCALE, ch) for ch in range(NCH)] + \
                    [(wk, "wk", kT, "kT", gk, 0.0, ch) for ch in range(NCH)]
            nvg = NTL // 4
            vdone = 0
            pend = None
            for ci, (wt, nm, dst, dkey, gain, extra, ch) in enumerate(plist):
                proj_mm(ci, wt, nm, ch)
                if pend is not None:
                    proj_norm(*pend)
                pend = (ci, dst, dkey, gain, extra, ch)
                want = ((ci + 1) * nvg) // len(plist)
                while vdone < want:
                    v_group(vdone)
                    vdone += 1
            proj_norm(*pend)
            while vdone < nvg:
                v_group(vdone)
                vdone += 1

            def scores(pos, r, kb):
                nq = 256 if kb + 1 < nb else 128
                k0 = Dg * 128 * kb + r
                pi = pos % 3
                bi = pos % 2
                bank = pst[bi]
                S.pe(lambda e, bank=bank, k0=k0, Dg=Dg, nq=nq: e.matmul(
                    bank[:, 0:nq], lhsT=kT[:, k0:k0 + 127 * Dg + 1:Dg], rhs=qT[:, k0:k0 + (nq - 1) * Dg + 1:Dg],
                    start=True, stop=False), reads=["kT", "qT"], writes=[("pq", bi)])
                S.pe(lambda e, bank=bank, nq=nq: e.matmul(
                    bank[:, 0:nq], lhsT=idb[:], rhs=mneg[:, 0:nq], start=False, stop=True),
                    reads=["idb", "mneg"], writes=[("pq", bi)])
                S.act(lambda e, bank=bank, pi=pi, nq=nq: e.activation(out=PT[pi][:, 0:nq], in_=bank[:, 0:nq], func=AF.Exp),
                      reads=[("pq", bi)], writes=[("PT", pi)])

            grp = [0]

            def pvacc(pos, r, qb):
                srcs = []
                if qb >= 1:
                    srcs.append((r * nb + qb - 1, (pos - 1) % 3, slice(128, 256)))
                srcs.append((r * nb + qb, pos % 3, slice(0, 128)))
                q4 = qb % 4
                g2 = grp[0] % 2
                for dst_ps, dkey, use_v in ((po[g2], ("po", g2), True), (pl[g2], ("pl", g2), False)):
                    for si, (vblk, pi, csl) in enumerate(srcs):
                        S.pe(lambda e, dst_ps=dst_ps, use_v=use_v, vblk=vblk, pi=pi, csl=csl, q4=q4, si=si, n=len(srcs): e.matmul(
                            dst_ps[:, q4, :], lhsT=(V[:, vblk, :] if use_v else onesb[:]), rhs=PT[pi][:, csl],
                            start=(si == 0), stop=(si == n - 1)),
                            reads=["V", "onesb", ("PT", pi)], writes=[dkey])
                if q4 == 3 or qb == nb - 1:
                    nqb = q4 + 1
                    qb0 = qb - q4
                    t0 = Dg * 128 * qb0 + r
                    tsl = slice(t0, t0 + (nqb * 128 - 1) * Dg + 1, Dg)
                    for dst_ps, dkey, acc, akey in ((po[g2], ("po", g2), oacc, "oacc"), (pl[g2], ("pl", g2), lacc, "lacc")):
                        src = dst_ps[:, 0:nqb, :].rearrange("p a b -> p (a b)")
                        if gi == 0:
                            S.dve(lambda e, src=src, acc=acc, tsl=tsl: e.tensor_copy(out=acc[:, tsl], in_=src),
                                  reads=[dkey], writes=[akey])
                        else:
                            S.dve(lambda e, src=src, acc=acc, tsl=tsl: e.tensor_tensor(
                                out=acc[:, tsl], in0=src, in1=acc[:, tsl], op=ALU.add),
                                reads=[dkey, akey], writes=[akey])
                    grp[0] += 1

            seq = [(r, kb) for r in range(Dg) for kb in range(nb)]
            prev = None
            for pos, (r, kb) in enumerate(seq):
                scores(pos, r, kb)
                if prev is not None:
                    pvacc(*prev)
                prev = (pos, r, kb)
            pvacc(*prev)
            if gi == 2:
                S.act(lambda e: e.activation(out=lacc[:], in_=lacc[:], func=AF.Ln), reads=["lacc"], writes=["lacc"])
                S.act(lambda e: e.activation(out=lacc[:], in_=lacc[:], func=AF.Exp, scale=-1.0), reads=["lacc"], writes=["lacc"])
                S.dve(lambda e: e.tensor_tensor(out=onb[:], in0=oacc[:], in1=lacc[:], op=ALU.mult),
                      reads=["oacc", "lacc"], writes=["onb"])
                S.dma("sp", "ost", onT[h * 128:(h + 1) * 128, :], onb[:], reads=["onb"])
        S.emit(final_chans=["ost"])


def phase_swa_out(nc, x_src, x_dst, onT, w_out, S_len):
    import contextlib
    NTL = S_len // 128
    with contextlib.ExitStack() as es:
        C = Ctx(nc, es)
        S = C.S
        wout_sb = C.sb("wout", [128, 8, D], BF16)
        ot = [C.sb("ot%d" % i, [128, 8, 128], BF16) for i in range(2)]
        xr = [C.sb("xr%d" % i, [128, D], F32) for i in range(2)]
        pp = [C.ps("pp%d" % i, [128, 512], F32) for i in range(4)]
        S.psum_keys.update([("pp", i) for i in range(4)])
        S.dma("pool", "w_out", wout_sb[:], w_out.rearrange("(kc p) n -> p kc n", p=128), writes=["wout"])
        onT_v = onT.rearrange("(h p) t -> p h t", p=128)
        for j in range(NTL):
            s = j % 2
            S.dma("sp", "ot%d" % s, ot[s][:], onT_v[:, :, j * 128:(j + 1) * 128], writes=[("ot", s)])
            S.dma("sp", "xr%d" % s, xr[s][:], x_src[j * 128:(j + 1) * 128, :], writes=[("xr", s)])
            for half in range(2):
                bi = (2 * j + half) % 4
                bank = pp[bi]
                for c8 in range(8):
                    S.pe(lambda e, c8=c8, half=half, bank=bank, s=s: e.matmul(
                        bank[:], lhsT=ot[s][:, c8, :], rhs=wout_sb[:, c8, half * 512:(half + 1) * 512],
                        start=(c8 == 0), stop=(c8 == 7)), reads=[("ot", s), "wout"], writes=[("pp", bi)])
                S.dve(lambda e, s=s, half=half, bank=bank: e.tensor_tensor(
                    out=xr[s][:, half * 512:(half + 1) * 512], in0=bank[:],
                    in1=xr[s][:, half * 512:(half + 1) * 512], op=ALU.add),
                    reads=[("pp", bi), ("xr", s)], writes=[("xr", s)])
            S.dma("pool", "xs%d" % s, x_dst[j * 128:(j + 1) * 128, :], xr[s][:], reads=[("xr", s)])
        S.emit(final_chans=["xs0", "xs1"])


DEPTH = 4


def build_program(S_len):
    nc = bass.Bass("TRN2", target_bir_lowering=False)

    def inp(name, shape):
        return nc.dram_tensor(name, shape, F32, kind="ExternalInput").ap()

    x = inp("x", [S_len, D])
    norm_mix = inp("norm_mix", [4, 128, 8])
    norm_mlp = inp("norm_mlp", [4, 128, 8])
    gla_w_in = inp("gla_w_in", [2, D, GLA_IN])
    gla_w_gate = inp("gla_w_gate_up", [2, 16, 512])
    gla_b_gate = inp("gla_b_gate", [2, 1, 512])
    gla_g_out = inp("gla_g_out", [2, 1, 1024])
    gla_w_out = inp("gla_w_out", [2, D, D])
    swa_w_qkv = inp("swa_w_qkv", [2, D, SWA_IN])
    swa_g_q = inp("swa_g_q", [2, 128, 3])
    swa_g_k = inp("swa_g_k", [2, 128, 3])
    swa_w_out = inp("swa_w_out", [2, D, D])
    mlp_w_up = inp("mlp_w_up", [4, D, DFF])
    mlp_w_down = inp("mlp_w_down", [4, DFF, D])
    ident = inp("c_ident", [128, 128])
    tri = inp("c_tri", [128, 128])
    trineg = inp("c_trineg", [128, 128])
    maskneg = inp("c_maskneg", [128, 256])
    out = nc.dram_tensor("out", [S_len, D], F32, kind="ExternalOutput").ap()
    onT = nc.dram_tensor("onT_scratch", [D, S_len], BF16).ap()
    src = x
    for i in range(DEPTH):
        j = i // 2
        if i % 2 == 0:
            phase_gla(nc, src, out, norm_mix[i], gla_w_in[j], gla_w_gate[j], gla_b_gate[j], gla_g_out[j],
                      gla_w_out[j], ident, tri, trineg, S_len)
        else:
            phase_swa_attn(nc, src, onT, norm_mix[i], swa_w_qkv[j], swa_g_q[j], swa_g_k[j], ident, maskneg, S_len)
            phase_swa_out(nc, src, out, onT, swa_w_out[j], S_len)
        src = out
        phase_mlp(nc, src, out, norm_mlp[i], mlp_w_up[i], mlp_w_down[i], ident, S_len)
    return nc


def make_consts():
    jj, ii = np.meshgrid(np.arange(128), np.arange(128), indexing="ij")
    tri = (jj <= ii).astype(np.float32)
    i2, c2 = np.meshgrid(np.arange(128), np.arange(256), indexing="ij")
    mk = np.where((c2 >= i2) & (c2 <= i2 + 128), 0.0, -30000.0).astype(np.float32)
    return {"c_ident": np.eye(128, dtype=np.float32), "c_tri": tri, "c_trineg": (-tri / 16.0).astype(np.float32),
            "c_maskneg": mk}


def layout_inputs(inputs):
    f = lambda a: np.ascontiguousarray(np.asarray(a, dtype=np.float32))
    shared = {
        "norm_mix": f(np.asarray(inputs["norm_mix"]).reshape(-1, 8, 128).transpose(0, 2, 1)),
        "norm_mlp": f(np.asarray(inputs["norm_mlp"]).reshape(-1, 8, 128).transpose(0, 2, 1)),
        "gla_w_in": f(inputs["gla_w_in"]),
        "gla_w_gate_up": f(inputs["gla_w_gate_up"]),
        "gla_b_gate": f(np.asarray(inputs["gla_b_gate"]).reshape(2, 1, 512)),
        "gla_g_out": f(np.asarray(inputs["gla_g_out"]).reshape(2, 1, 1024)),
        "gla_w_out": f(inputs["gla_w_out"]),
        "swa_w_qkv": f(inputs["swa_w_qkv"]),
        "swa_g_q": f(np.asarray(inputs["swa_g_q"]).transpose(0, 2, 1)),
        "swa_g_k": f(np.asarray(inputs["swa_g_k"]).transpose(0, 2, 1)),
        "swa_w_out": f(inputs["swa_w_out"]),
        "mlp_w_up": f(inputs["mlp_w_up"]),
        "mlp_w_down": f(inputs["mlp_w_down"]),
    }
    shared.update(make_consts())
    return shared


def kernel(**inputs):
    x = np.asarray(inputs["x"], dtype=np.float32)
    B, S_len, _ = x.shape
    shared = layout_inputs(inputs)
    nc = build_program(S_len)
    in_maps = []
    for b in range(B):
        m = dict(shared)
        m["x"] = np.ascontiguousarray(x[b])
        in_maps.append(m)
    res = run_bass_kernel_spmd(nc, in_maps, core_ids=list(range(B)))
    return np.stack([np.asarray(r["out"], dtype=np.float32) for r in res.results], axis=0)
```

```python
import numpy as np
import concourse.bass as bass
import concourse.mybir as mybir
from concourse.bass_utils import run_bass_kernel_spmd

F32 = mybir.dt.float32
BF16 = mybir.dt.bfloat16
AF = mybir.ActivationFunctionType
ALU = mybir.AluOpType
AX = mybir.AxisListType

SAME_ENGINE_SYNC = True


class Sched:
    ENGS = ("pe", "act", "dve", "pool", "sp")
    _semn = [0]

    def __init__(self, nc):
        self.nc = nc
        self.ops = {e: [] for e in self.ENGS}
        self.last_w = {}
        self.readers = {}
        self.chan_cnt = {}
        self.seen = {e: {} for e in self.ENGS}
        self.epoch = 0
        self.needs_inc = set()
        self.psum_keys = set()
        self.last_x = {}

    def new_epoch(self):
        self.epoch += 1

    def add(self, eng, fn, reads=(), writes=(), chan=None):
        idx = len(self.ops[eng])
        deps = []
        for k in reads:
            t = self.last_w.get(k)
            if t is not None:
                deps.append(t)
        for k in writes:
            t = self.last_w.get(k)
            if t is not None and not (chan is not None and t[0] == "d" and t[1] == chan):
                deps.append(t)
            deps.extend(self.readers.get(k, {}).values())
        xkeys = [k for k in tuple(reads) + tuple(writes) if k in self.psum_keys]
        for k in xkeys:
            t = self.last_x.get(k)
            if t is not None and t[1][0] != eng:
                deps.append(t)
        if chan is not None:
            c = self.chan_cnt.get(chan, 0) + 1
            self.chan_cnt[chan] = c
            tok = ("d", chan, c)
        else:
            tok = ("c", (eng, self.epoch), idx)
        best = {}
        for t in deps:
            kind, sk, v = t
            if kind == "c":
                if sk[0] == eng and (eng == "pe" or not SAME_ENGINE_SYNC):
                    continue
                if sk[0] == eng and v >= idx:
                    continue
            key = (kind, sk)
            if best.get(key, -1) < v:
                best[key] = v
        waits = []
        seen = self.seen[eng]
        for key, v in best.items():
            if seen.get(key, -1) >= v:
                continue
            seen[key] = v
            waits.append((key[0], key[1], v))
            if key[0] == "c":
                self.needs_inc.add((key[1], v))
        self.ops[eng].append(dict(fn=fn, waits=waits, chan=chan, epoch=self.epoch))
        for k in xkeys:
            self.last_x[k] = tok
        for k in writes:
            self.last_w[k] = tok
            self.readers[k] = {}
        for k in reads:
            self.readers.setdefault(k, {})[(tok[0], tok[1])] = tok
        return tok

    def pe(self, fn, reads=(), writes=()):
        return self.add("pe", fn, reads, writes)

    def act(self, fn, reads=(), writes=()):
        return self.add("act", fn, reads, writes)

    def dve(self, fn, reads=(), writes=()):
        return self.add("dve", fn, reads, writes)

    def pool(self, fn, reads=(), writes=()):
        return self.add("pool", fn, reads, writes)

    def dma(self, queue, chan, out, in_, reads=(), writes=()):
        return self.add(queue, lambda e: e.dma_start(out=out, in_=in_), reads, writes, chan=chan)

    def emit(self, final_chans=()):
        nc = self.nc
        last_c = {}
        for e in self.ENGS:
            for idx in range(len(self.ops[e]) - 1, -1, -1):
                if self.ops[e][idx]["chan"] is None:
                    sk = (e, self.ops[e][idx]["epoch"])
                    self.needs_inc.add((sk, idx))
                    last_c[e] = (sk, idx)
                    break
        cnt = {}
        incval = {}
        for e in self.ENGS:
            for idx, op in enumerate(self.ops[e]):
                sk = (e, op["epoch"])
                if (sk, idx) in self.needs_inc:
                    cnt[sk] = cnt.get(sk, 0) + 1
                    incval[(sk, idx)] = cnt[sk]
        sem_keys = sorted(cnt.keys()) + sorted(("chan", c) for c in self.chan_cnt)
        sems = {}
        for k in sem_keys:
            Sched._semn[0] += 1
            sems[k] = nc.alloc_semaphore("sem%d" % Sched._semn[0])
        with nc.Block() as block:

            def run(ename, eng):
                for idx, op in enumerate(self.ops[ename]):
                    for kind, sk, v in op["waits"]:
                        if kind == "c":
                            eng.wait_ge(sems[sk], incval[(sk, v)])
                        else:
                            eng.wait_ge(sems[("chan", sk)], 16 * v)
                    ins = op["fn"](eng)
                    sk = (ename, op["epoch"])
                    if op["chan"] is not None:
                        ins.then_inc(sems[("chan", op["chan"])], 16)
                    elif (sk, idx) in incval:
                        ins.then_inc(sems[sk], 1)
                for e2, (sk, idx) in last_c.items():
                    if e2 != ename:
                        eng.wait_ge(sems[sk], incval[(sk, idx)])
                for c, n in self.chan_cnt.items():
                    eng.wait_ge(sems[("chan", c)], 16 * n)

            @block.tensor
            def _(e):
                run("pe", e)

            @block.scalar
            def _(e):
                run("act", e)

            @block.vector
            def _(e):
                run("dve", e)

            @block.gpsimd
            def _(e):
                run("pool", e)

            @block.sync
            def _(e):
                run("sp", e)
        nc.all_engine_barrier()
        nc.clear_and_free_semaphores(list(sems.values()))
        nc.all_engine_barrier()

    def barrier_deps(self):
        return None


D = 1024
DFF = 4096
EPS = 1e-6
NT = 128


class Ctx:
    _n = [0]

    def __init__(self, nc, es):
        self.nc = nc
        self.es = es
        self.S = Sched(nc)
        Ctx._n[0] += 1
        self.tag = "p%d_" % Ctx._n[0]

    def sb(self, name, shape, dt):
        return self.es.enter_context(self.nc.sbuf_tensor(self.tag + name, shape, dt))

    def ps(self, name, shape, dt):
        return self.es.enter_context(self.nc.psum_tensor(self.tag + name, shape, dt))


def emit_norm_tile(C, xin, xkey, hb, hbkey, junk, st, stkey):
    S = C.S
    S.pool(lambda e: e.memset(st[:, 0:1], 0.0), writes=[stkey])
    S.act(lambda e: e.activation(out=junk[:], in_=xin[:], func=AF.Square, scale=1.0 / 32.0,
                                 accum_out=st[:, 0:1]), reads=[xkey, stkey], writes=["junk", stkey])
    S.act(lambda e: e.activation(out=st[:, 1:2], in_=st[:, 0:1], func=AF.Sqrt, bias=EPS, scale=1.0),
          reads=[stkey], writes=[stkey])
    S.dve(lambda e: e.reciprocal(out=st[:, 2:3], in_=st[:, 1:2]), reads=[stkey], writes=[stkey])
    S.act(lambda e: e.activation(out=hb[:], in_=xin[:], func=AF.Copy, scale=st[:, 2:3]),
          reads=[xkey, stkey], writes=[hbkey])


def emit_transpose_tile(C, hb, hbkey, idb, ptr, ptrkey, hT_dst, hTkey, gcol):
    S = C.S
    for c in range(8):
        S.pe(lambda e, c=c: e.transpose(out=ptr[:, c, :], in_=hb[:, c * 128:(c + 1) * 128], identity=idb[:]),
             reads=[hbkey, "idb"], writes=[ptrkey])
    S.dve(lambda e: e.tensor_tensor(out=hT_dst, in0=ptr[:, :, :],
                                    in1=gcol[:, :].unsqueeze(2).to_broadcast([128, 8, 128]), op=ALU.mult),
          reads=[ptrkey, "gcol"], writes=[hTkey])


def phase_mlp(nc, x_src, x_dst, g_ap, wup, wdn, ident, S_len):
    import contextlib
    M = S_len // 512
    with contextlib.ExitStack() as es:
        C = Ctx(nc, es)
        S = C.S
        wup_sb = C.sb("wup", [128, 8, DFF], BF16)
        wdn_sb = C.sb("wdn", [128, 32, D], BF16)
        idb = C.sb("idb", [128, 128], BF16)
        gcol = C.sb("gcol", [128, 8], F32)
        xn = [C.sb("xn%d" % i, [128, D], F32) for i in range(2)]
        hb = [C.sb("hb%d" % i, [128, D], BF16) for i in range(4)]
        st = [C.sb("st%d" % i, [128, 4], F32) for i in range(2)]
        junk = C.sb("junk", [128, D], BF16)
        hT = [C.sb("hT%d" % i, [128, 8, 512], BF16) for i in range(2)]
        actT = C.sb("actT", [128, 32, 512], BF16)
        rl = [C.sb("rl%d" % i, [128, 512], F32) for i in range(2)]
        xr = [C.sb("xr%d" % i, [128, D], F32) for i in range(2)]
        pu = [C.ps("pu%d" % i, [128, 512], F32) for i in range(3)]
        pd = [C.ps("pd%d" % i, [128, 512], F32) for i in range(4)]
        ptr = C.ps("ptr", [128, 8, 128], BF16)
        S.psum_keys.update([("pu", i) for i in range(3)] + [("pd", i) for i in range(4)] + ["ptr"])

        S.dma("pool", "c_id", idb[:], ident, writes=["idb"])
        S.dma("sp", "c_g", gcol[:], g_ap, writes=["gcol"])
        wup_v = wup.rearrange("(kc p) n -> p kc n", p=128)
        wdn_v = wdn.rearrange("(fc p) n -> p fc n", p=128)
        for g in range(8):
            S.dma("pool", "w_up%d" % g, wup_sb[:, :, g * 512:(g + 1) * 512], wup_v[:, :, g * 512:(g + 1) * 512],
                  writes=[("wup", g)])
        for g in range(8):
            S.dma("pool", "w_dn%d" % g, wdn_sb[:, g * 4:(g + 1) * 4, :], wdn_v[:, g * 4:(g + 1) * 4, :],
                  writes=[("wdn", g)])

        def load_xn(j):
            s = j % 2
            S.dma("sp", "xn%d" % s, xn[s][:], x_src[j * 128:(j + 1) * 128, :], writes=[("xn", s)])

        def norm_elem(j):
            s = j % 2
            emit_norm_tile(C, xn[s], ("xn", s), hb[j % 4], ("hb", j % 4), junk, st[s], ("st", s))

        def transp(j):
            m, q = divmod(j, 4)
            emit_transpose_tile(C, hb[q], ("hb", q), idb, ptr, "ptr",
                                hT[m % 2][:, :, q * 128:(q + 1) * 128], ("hT", m % 2), gcol)

        for q in range(4):
            load_xn(q)
            norm_elem(q)
            transp(q)
        for m in range(M):
            ms = m % 2
            for g in range(32):
                bank = pu[g % 3]
                for kc in range(8):
                    S.pe(lambda e, g=g, kc=kc, bank=bank, ms=ms: e.matmul(
                        bank[:], lhsT=wup_sb[:, kc, g * 128:(g + 1) * 128], rhs=hT[ms][:, kc, :],
                        start=(kc == 0), stop=(kc == 7)),
                        reads=[("wup", g // 4), ("hT", ms)], writes=[("pu", g % 3)])
                r = rl[g % 2]
                S.act(lambda e, bank=bank, r=r: e.activation(out=r[:], in_=bank[:], func=AF.Relu),
                      reads=[("pu", g % 3)], writes=[("rl", g % 2)])
                S.dve(lambda e, g=g, r=r: e.tensor_tensor(out=actT[:, g, :], in0=r[:], in1=r[:], op=ALU.mult),
                      reads=[("rl", g % 2)], writes=["actT"])
                if m + 1 < M and g % 8 == 1:
                    q = g // 8
                    load_xn((m + 1) * 4 + q)
                    norm_elem((m + 1) * 4 + q)
            if m + 1 < M:
                for q in range(4):
                    transp((m + 1) * 4 + q)
            for q in range(4):
                j = m * 4 + q
                s = j % 2
                S.dma("pool", "xr%d" % s, xr[s][:], x_src[j * 128:(j + 1) * 128, :], writes=[("xr", s)])
                for half in range(2):
                    bank = pd[(2 * q + half) % 4]
                    bk = ("pd", (2 * q + half) % 4)
                    for fc in range(32):
                        S.pe(lambda e, fc=fc, q=q, half=half, bank=bank: e.matmul(
                            bank[:], lhsT=actT[:, fc, q * 128:(q + 1) * 128],
                            rhs=wdn_sb[:, fc, half * 512:(half + 1) * 512],
                            start=(fc == 0), stop=(fc == 31)),
                            reads=["actT", ("wdn", fc // 4)], writes=[bk])
                    S.dve(lambda e, s=s, half=half, bank=bank: e.tensor_tensor(
                        out=xr[s][:, half * 512:(half + 1) * 512], in0=bank[:],
                        in1=xr[s][:, half * 512:(half + 1) * 512], op=ALU.add),
                        reads=[bk, ("xr", s)], writes=[("xr", s)])
                S.dma("pool", "xs%d" % s, x_dst[j * 128:(j + 1) * 128, :], xr[s][:], reads=[("xr", s)])
        S.emit(final_chans=["xs0", "xs1"])


GLA_H = 4
STAGE = 99
GLA_IN = 3088


def emit_rstd(C, st, stkey, n):
    S = C.S
    S.act(lambda e: e.activation(out=st[:, n:2 * n], in_=st[:, 0:n], func=AF.Ln, bias=EPS, scale=1.0),
          reads=[stkey], writes=[stkey])
    S.act(lambda e: e.activation(out=st[:, 2 * n:3 * n], in_=st[:, n:2 * n], func=AF.Exp, scale=-0.5),
          reads=[stkey], writes=[stkey])


def emit_norm_tile2(C, xin, xkey, hb, hbkey, junk, st, stkey):
    S = C.S
    S.pool(lambda e: e.memset(st[:, 0:1], 0.0), writes=[stkey])
    S.act(lambda e: e.activation(out=junk[:], in_=xin[:], func=AF.Square, scale=1.0 / 32.0,
                                 accum_out=st[:, 0:1]), reads=[xkey, stkey], writes=["junk", stkey])
    emit_rstd(C, st, stkey, 1)
    S.act(lambda e: e.activation(out=hb[:], in_=xin[:], func=AF.Copy, scale=st[:, 2:3]),
          reads=[xkey, stkey], writes=[hbkey])


def phase_gla(nc, x_src, x_dst, g_ap, w_in, w_gate, b_gate, g_out, w_out, ident, tri, trineg, S_len):
    import contextlib
    M = S_len // 512
    with contextlib.ExitStack() as es:
        C = Ctx(nc, es)
        S = C.S
        win_sb = C.sb("win", [128, 8, GLA_IN], BF16)
        wout_sb = C.sb("wout", [128, 8, D], BF16)
        idb = C.sb("idb", [128, 128], BF16)
        gcol = C.sb("gcol", [128, 8], F32)
        trim = C.sb("trim", [128, 128], F32)
        trin = C.sb("trin", [128, 128], BF16)
        wg_sb = C.sb("wg", [128, 512], F32)
        wg_hi = C.sb("wg_hi", [128, 512], BF16)
        wg_lo = C.sb("wg_lo", [128, 512], BF16)
        zT_hi = C.sb("zT_hi", [128, 512], BF16)
        zT_lo = C.sb("zT_lo", [128, 512], BF16)
        goutB = C.sb("goutB", [128, D], F32)
        xn = [C.sb("xn%d" % i, [128, D], F32) for i in range(2)]
        hb = [C.sb("hb0", [128, D], BF16)]
        st = [C.sb("st%d" % i, [128, 4], F32) for i in range(2)]
        junk = C.sb("junk", [128, D], BF16)
        hT = C.sb("hT", [128, 8, 512], BF16)
        qTf = C.sb("qTf", [128, 4, 512], F32)
        kTf = C.sb("kTf", [128, 4, 512], F32)
        zT = C.sb("zT", [128, 512], F32)
        v_sb = C.sb("v", [128, 4, D], BF16)
        r_sb = C.sb("r", [128, D], F32)
        sr = C.sb("sr", [128, 4, D], F32)
        et = C.sb("et", [128, D], F32)
        glf = C.sb("glf", [128, 512], F32)
        gl_hi = C.sb("gl_hi", [128, 4, 512], BF16)
        gl_lo = C.sb("gl_lo", [128, 4, 512], BF16)
        eb = [C.sb("eb%d" % i, [128, 4, 128], F32) for i in range(2)]
        enb = C.sb("enb", [128, 4, 128], F32)
        qt = [C.sb("qt%d" % i, [128, 4, 128], BF16) for i in range(2)]
        kt = [C.sb("kt%d" % i, [128, 4, 128], BF16) for i in range(2)]
        ktok = C.sb("ktok", [128, 4, 128], BF16)
        AT = C.sb("AT", [128, 4, 128], BF16)
        St = C.sb("St", [128, 4, 256], F32)
        Sbf = [C.sb("Sbf%d" % i, [128, 4, 256], BF16) for i in range(2)]
        ost = C.sb("ost", [128, 12], F32)
        on = C.sb("on", [128, 4, 256], F32)
        gated = C.sb("gated", [128, D], BF16)
        gT = C.sb("gT", [128, 8, 128], BF16)
        xr = [C.sb("xr%d" % i, [128, D], F32) for i in range(2)]
        pp = [C.ps("pp%d" % i, [128, 512], F32) for i in range(2)]
        ptr = C.ps("ptr", [128, 8, 128], BF16)
        pba = C.ps("pba", [128, 4, 128], F32)
        po = C.ps("po", [128, 4, 256], F32)
        pS = C.ps("pS", [128, 4, 256], F32)
        S.psum_keys.update([("pp", 0), ("pp", 1), "ptr", "pba", "po", "pS"])

        S.dma("pool", "c_id", idb[:], ident, writes=["idb"])
        S.dma("sp", "c_g", gcol[:], g_ap, writes=["gcol"])
        S.dma("sp", "c_t1", trim[:], tri, writes=["trim"])
        S.dma("pool", "c_t2", trin[:], trineg, writes=["trin"])
        S.pool(lambda e: e.memset(wg_sb[:], 0.0), writes=["wg"])
        S.dma("sp", "c_wg", wg_sb[0:16, :], w_gate, writes=["wg"])
        S.dma("sp", "c_wg", wg_sb[16:17, :], b_gate, writes=["wg"])
        S.dve(lambda e: e.tensor_copy(out=wg_hi[:], in_=wg_sb[:]), reads=["wg"], writes=["wg_hi"])
        S.dve(lambda e: e.tensor_tensor(out=wg_lo[:], in0=wg_sb[:], in1=wg_hi[:], op=ALU.subtract),
              reads=["wg", "wg_hi"], writes=["wg_lo"])
        S.pool(lambda e: e.memset(zT[:], 0.0), writes=["zT"])
        S.pool(lambda e: e.memset(zT[0:32, :], 1.0), writes=["zT"])
        S.dma("sp", "c_go", goutB[:], g_out.partition_broadcast(128), writes=["goutB"])
        S.pool(lambda e: e.memset(St[:], 0.0), writes=["St"])
        S.pool(lambda e: e.memset(Sbf[0][:], 0.0), writes=[("Sbf", 0)])
        win_v = w_in.rearrange("(kc p) n -> p kc n", p=128)
        wout_v = w_out.rearrange("(kc p) n -> p kc n", p=128)
        blocks = [(0, 1024), (1024, 2048), (2048, 3072), (3072, 3088)]
        for bi, (c0, c1) in enumerate(blocks):
            S.dma("pool", "w_in%d" % bi, win_sb[:, :, c0:c1], win_v[:, :, c0:c1], writes=[("win", bi)])
        S.dma("pool", "w_out", wout_sb[:], wout_v, writes=["wout"])

        pcnt = [0]

        def pbank():
            i = pcnt[0] % 2
            pcnt[0] += 1
            return pp[i], ("pp", i)

        def P0(m):
            for q in range(4):
                j = m * 4 + q
                s = j % 2
                S.dma("sp", "xn%d" % s, xn[s][:], x_src[j * 128:(j + 1) * 128, :], writes=[("xn", s)])
                emit_norm_tile2(C, xn[s], ("xn", s), hb[0], ("hb", 0), junk, st[s], ("st", s))
                emit_transpose_tile(C, hb[0], ("hb", 0), idb, ptr, "ptr",
                                    hT[:, :, q * 128:(q + 1) * 128], "hT", gcol)
            bank, bk = pbank()
            for kc in range(8):
                S.pe(lambda e, kc=kc, bank=bank: e.matmul(
                    bank[0:16, :], lhsT=win_sb[:, kc, 3072:3088], rhs=hT[:, kc, :],
                    start=(kc == 0), stop=(kc == 7)), reads=[("win", 3), "hT"], writes=[bk])
            S.dve(lambda e, bank=bank: e.tensor_copy(out=zT[0:16, :], in_=bank[0:16, :]), reads=[bk], writes=["zT"])
            S.dve(lambda e: e.tensor_copy(out=zT_hi[:], in_=zT[:]), reads=["zT"], writes=["zT_hi"])
            S.dve(lambda e: e.tensor_tensor(out=zT_lo[:], in0=zT[:], in1=zT_hi[:], op=ALU.subtract),
                  reads=["zT", "zT_hi"], writes=["zT_lo"])

        def Pqk(m, gis=range(8)):
            for gi in gis:
                bank, bk = pbank()
                for kc in range(8):
                    S.pe(lambda e, gi=gi, kc=kc, bank=bank: e.matmul(
                        bank[:], lhsT=win_sb[:, kc, gi * 128:(gi + 1) * 128], rhs=hT[:, kc, :],
                        start=(kc == 0), stop=(kc == 7)), reads=[("win", 0), "hT"], writes=[bk])
                if gi < 4:
                    S.act(lambda e, gi=gi, bank=bank: e.activation(out=qTf[:, gi, :], in_=bank[:], func=AF.Copy,
                                                                   scale=128.0 ** -0.5),
                          reads=[bk], writes=["qTf"])
                else:
                    S.dve(lambda e, gi=gi, bank=bank: e.tensor_copy(out=kTf[:, gi - 4, :], in_=bank[:]),
                          reads=[bk], writes=["kTf"])

        def Pv(m, q):
            for half in range(2):
                bank, bk = pbank()
                for kc in range(8):
                    S.pe(lambda e, q=q, half=half, kc=kc, bank=bank: e.matmul(
                        bank[:], lhsT=hT[:, kc, q * 128:(q + 1) * 128],
                        rhs=win_sb[:, kc, 1024 + half * 512:1024 + (half + 1) * 512],
                        start=(kc == 0), stop=(kc == 7)), reads=[("win", 1), "hT"], writes=[bk])
                S.dve(lambda e, q=q, half=half, bank=bank: e.tensor_copy(
                    out=v_sb[:, q, half * 512:(half + 1) * 512], in_=bank[:]), reads=[bk], writes=[("v", q)])
            for half in range(2):
                bank, bk = pbank()
                for kc in range(8):
                    S.pe(lambda e, q=q, half=half, kc=kc, bank=bank: e.matmul(
                        bank[:], lhsT=hT[:, kc, q * 128:(q + 1) * 128],
                        rhs=win_sb[:, kc, 2048 + half * 512:2048 + (half + 1) * 512],
                        start=(kc == 0), stop=(kc == 7)), reads=[("win", 2), "hT"], writes=[bk])
                hs = slice(half * 512, (half + 1) * 512)
                S.act(lambda e, hs=hs, bank=bank: e.activation(out=et[:, hs], in_=bank[:], func=AF.Exp, scale=-1.0),
                      reads=[bk], writes=["et"])
                S.dve(lambda e, hs=hs, bank=bank: e.tensor_copy(out=r_sb[:, hs], in_=bank[:]),
                      reads=[bk], writes=["r"])
            S.act(lambda e: e.activation(out=et[:], in_=et[:], func=AF.Ln, bias=1.0, scale=1.0),
                  reads=["et"], writes=["et"])
            S.act(lambda e, q=q: e.activation(out=sr[:, q, :], in_=et[:], func=AF.Exp, scale=-1.0),
                  reads=["et"], writes=[("sr", q)])
            S.pool(lambda e, q=q: e.tensor_tensor(out=sr[:, q, :], in0=sr[:, q, :], in1=r_sb[:, :], op=ALU.mult),
                   reads=[("sr", q), "r"], writes=[("sr", q)])
            S.pool(lambda e, q=q: e.tensor_tensor(out=sr[:, q, :], in0=sr[:, q, :], in1=goutB[:], op=ALU.mult),
                   reads=[("sr", q), "goutB"], writes=[("sr", q)])
            bank, bk = pbank()
            qs = slice(q * 128, (q + 1) * 128)
            for pi, (za, wa) in enumerate(((zT_hi, wg_hi), (zT_lo, wg_hi), (zT_hi, wg_lo))):
                S.pe(lambda e, qs=qs, bank=bank, za=za, wa=wa, pi=pi: e.matmul(
                    bank[:], lhsT=za[:, qs], rhs=wa[:, :], start=(pi == 0), stop=(pi == 2)),
                    reads=["zT_hi", "zT_lo", "wg_hi", "wg_lo"], writes=[bk])
            S.act(lambda e, bank=bank: e.activation(out=glf[:], in_=bank[:], func=AF.Exp, scale=-1.0),
                  reads=[bk], writes=["glf"])
            S.act(lambda e: e.activation(out=glf[:], in_=glf[:], func=AF.Ln, bias=1.0, scale=1.0),
                  reads=["glf"], writes=["glf"])
            S.dve(lambda e, q=q: e.tensor_copy(out=gl_hi[:, q, :], in_=glf[:]), reads=["glf"], writes=[("gl", q)])
            S.dve(lambda e, q=q: e.tensor_tensor(out=gl_lo[:, q, :], in0=glf[:], in1=gl_hi[:, q, :], op=ALU.subtract),
                  reads=["glf", ("gl", q)], writes=[("gl", q)])

        def Aa(m, q):
            e2 = q % 2
            ts = slice(q * 128, (q + 1) * 128)
            for h in range(4):
                for pi, ga in enumerate((gl_hi, gl_lo)):
                    S.pe(lambda e, q=q, h=h, ga=ga, pi=pi: e.matmul(
                        pba[:, h, :], lhsT=ga[:, q, h * 128:(h + 1) * 128], rhs=trin[:, :],
                        start=(pi == 0), stop=(pi == 1)), reads=[("gl", q), "trin"], writes=["pba"])
            S.act(lambda e, e2=e2: e.activation(out=eb[e2][:], in_=pba[:], func=AF.Exp), reads=["pba"], writes=[("eb", e2)])
            S.act(lambda e: e.activation(out=enb[:], in_=pba[:], func=AF.Exp, scale=-1.0), reads=["pba"], writes=["enb"])
            S.dve(lambda e, ts=ts, e2=e2: e.tensor_tensor(out=qt[e2][:], in0=qTf[:, :, ts], in1=eb[e2][:], op=ALU.mult),
                  reads=["qTf", ("eb", e2)], writes=[("qt", e2)])
            S.dve(lambda e, ts=ts, e2=e2: e.tensor_tensor(out=kt[e2][:], in0=kTf[:, :, ts], in1=enb[:], op=ALU.mult),
                  reads=["kTf", "enb"], writes=[("kt", e2)])

        def Ab(m, q):
            e2 = q % 2
            for h in range(4):
                S.pe(lambda e, h=h, e2=e2: e.transpose(out=ptr[:, h, :], in_=kt[e2][:, h, :], identity=idb[:]),
                     reads=[("kt", e2), "idb"], writes=["ptr"])
            S.act(lambda e: e.copy(out=ktok[:], in_=ptr[:, 0:4, :]), reads=["ptr"], writes=["ktok"])
            for h in range(4):
                S.pe(lambda e, h=h, e2=e2: e.matmul(pba[:, h, :], lhsT=kt[e2][:, h, :], rhs=qt[e2][:, h, :],
                                                    start=True, stop=True),
                     reads=[("kt", e2), ("qt", e2)], writes=["pba"])
            S.dve(lambda e: e.tensor_tensor(out=AT[:], in0=pba[:],
                                            in1=trim[:, :].unsqueeze(1).to_broadcast([128, 4, 128]), op=ALU.mult),
                  reads=["pba", "trim"], writes=["AT"])

        def Bs(m, q):
            e2 = q % 2
            c = m * 4 + q
            sb_in = Sbf[c % 2]
            so = Sbf[(c + 1) % 2]
            for h in range(4):
                S.pe(lambda e, q=q, h=h: e.matmul(po[:, h, :], lhsT=AT[:, h, :], rhs=v_sb[:, q, h * 256:(h + 1) * 256],
                                                  start=(h % 2 == 0), stop=False, skip_group_check=True),
                     reads=["AT", ("v", q)], writes=["po"])
            for h in range(4):
                S.pe(lambda e, h=h, e2=e2, sb_in=sb_in: e.matmul(
                    po[:, h, :], lhsT=qt[e2][:, h, :], rhs=sb_in[:, h, :], start=False, stop=True,
                    skip_group_check=True), reads=[("qt", e2), ("Sbf", c % 2)], writes=["po"])
            for h in range(4):
                S.pe(lambda e, q=q, h=h: e.matmul(
                    pS[:, h, :], lhsT=ktok[:, h, :], rhs=v_sb[:, q, h * 256:(h + 1) * 256],
                    start=True, stop=True), reads=["ktok", ("v", q)], writes=["pS"])
            S.dve(lambda e: e.tensor_tensor(out=St[:], in0=pS[:], in1=St[:], op=ALU.add),
                  reads=["pS", "St"], writes=["St"])
            S.dve(lambda e, e2=e2: e.tensor_tensor(
                out=St[:], in0=St[:], in1=eb[e2][:, :, 127:128].to_broadcast([128, 4, 256]), op=ALU.mult),
                reads=["St", ("eb", e2)], writes=["St"])
            S.act(lambda e, so=so: e.copy(out=so[:], in_=St[:]), reads=["St"], writes=[("Sbf", (c + 1) % 2)])

        def Ca(m, q):
            S.pool(lambda e: e.memset(ost[:, 0:4], 0.0), writes=["ost"])
            for h in range(4):
                S.act(lambda e, h=h: e.activation(out=junk[:, 0:256], in_=po[:, h, :], func=AF.Square, scale=1.0 / 16.0,
                                                  accum_out=ost[:, h:h + 1]), reads=["po", "ost"], writes=["junk", "ost"])
            emit_rstd(C, ost, "ost", 4)
            S.dve(lambda e: e.tensor_tensor(out=on[:], in0=po[:], in1=ost[:, 8:12].unsqueeze(2).to_broadcast([128, 4, 256]),
                                            op=ALU.mult), reads=["po", "ost"], writes=["on"])
            S.pool(lambda e, q=q: e.tensor_tensor(out=gated[:], in0=on[:].rearrange("p h v -> p (h v)"), in1=sr[:, q, :],
                                                  op=ALU.mult), reads=["on", ("sr", q)], writes=["gated"])

        def Cb(m, q):
            for c8 in range(8):
                S.pe(lambda e, c8=c8: e.transpose(out=ptr[:, c8, :], in_=gated[:, c8 * 128:(c8 + 1) * 128], identity=idb[:]),
                     reads=["gated", "idb"], writes=["ptr"])
            S.act(lambda e: e.copy(out=gT[:], in_=ptr[:]), reads=["ptr"], writes=["gT"])

        def Cc(m, q):
            j = m * 4 + q
            s = j % 2
            S.dma("pool", "xr%d" % s, xr[s][:], x_src[j * 128:(j + 1) * 128, :], writes=[("xr", s)])
            for half in range(2):
                bank, bk = pbank()
                for c8 in range(8):
                    S.pe(lambda e, c8=c8, half=half, bank=bank: e.matmul(
                        bank[:], lhsT=gT[:, c8, :], rhs=wout_sb[:, c8, half * 512:(half + 1) * 512],
                        start=(c8 == 0), stop=(c8 == 7)), reads=["gT", "wout"], writes=[bk])
                S.dve(lambda e, s=s, half=half, bank=bank: e.tensor_tensor(
                    out=xr[s][:, half * 512:(half + 1) * 512], in0=bank[:],
                    in1=xr[s][:, half * 512:(half + 1) * 512], op=ALU.add),
                    reads=[bk, ("xr", s)], writes=[("xr", s)])
            S.dma("pool", "xs%d" % s, x_dst[j * 128:(j + 1) * 128, :], xr[s][:], reads=[("xr", s)])


        P0(0)
        Pqk(0)
        for q in range(4):
            Pv(0, q)
        for m in range(M):
            nxt = m + 1 < M
            for n in range(4 + 3):
                if 0 <= n - 1 < 4:
                    Ab(m, n - 1)
                if 0 <= n - 3 < 4:
                    Cc(m, n - 3)
                if 0 <= n - 2 < 4:
                    Cb(m, n - 2)
                if 0 <= n - 1 < 4:
                    Bs(m, n - 1)
                    Ca(m, n - 1)
                if n < 4:
                    Aa(m, n)
                if nxt:
                    if n == 0:
                        P0(m + 1)
                    if 1 <= n <= 4:
                        Pv(m + 1, n - 1)
                    if n == 5:
                        Pqk(m + 1, range(0, 4))
                    if n == 6:
                        Pqk(m + 1, range(4, 8))

        S.emit(final_chans=["xs0", "xs1"])


SWA_PATTERNS = ((128, 1), (512, 4), (2048, 16))
SWA_H = 8
SWA_IN = 9216


def phase_swa_attn(nc, x_src, onT, g_ap, w_qkv, gq_ap, gk_ap, ident, maskneg, S_len):
    import contextlib
    import math
    NTL = S_len // 128
    NCH = S_len // 512
    with contextlib.ExitStack() as es:
        C = Ctx(nc, es)
        S = C.S
        hT = C.sb("hT", [128, 8, S_len], BF16)
        idb = C.sb("idb", [128, 128], BF16)
        onesb = C.sb("onesb", [128, 128], BF16)
        mneg = C.sb("mneg", [128, 256], BF16)
        gcol = C.sb("gcol", [128, 8], F32)
        gq = C.sb("gq", [128, 3], F32)
        gk = C.sb("gk", [128, 3], F32)
        xn = [C.sb("xn%d" % i, [128, D], F32) for i in range(2)]
        hb = [C.sb("hb%d" % i, [128, D], BF16) for i in range(2)]
        st = [C.sb("st%d" % i, [128, 4], F32) for i in range(2)]
        junk = C.sb("junk", [128, D], BF16)
        wq = [C.sb("wq%d" % i, [128, 8, 128], BF16) for i in range(2)]
        wk = [C.sb("wk%d" % i, [128, 8, 128], BF16) for i in range(2)]
        wv = [C.sb("wv%d" % i, [128, 8, 128], BF16) for i in range(2)]
        qT = C.sb("qT", [128, S_len], BF16)
        kT = C.sb("kT", [128, S_len], BF16)
        V = C.sb("V", [128, NTL, 128], BF16)
        raw = [C.sb("raw%d" % i, [128, 512], F32) for i in range(2)]
        sq = [C.sb("sq%d" % i, [128, 512], BF16) for i in range(2)]
        rs = [C.sb("rs%d" % i, [128, 512], F32) for i in range(2)]
        PT = [C.sb("PT%d" % i, [128, 256], BF16) for i in range(3)]
        oacc = C.sb("oacc", [128, S_len], F32)
        lacc = C.sb("lacc", [128, S_len], F32)
        onb = C.sb("onb", [128, S_len], BF16)
        pq = [C.ps("pq%d" % i, [128, 512], F32) for i in range(2)]
        pst = pq
        pss = C.ps("pss", [128, 512], F32)
        pvf = C.ps("pvf", [128, 4, 128], F32)
        pv = pvf[:].bitcast(BF16).rearrange("p a (b c) -> p (a b) c", c=128)
        po = [C.ps("po%d" % i, [128, 4, 128], F32) for i in range(2)]
        pl = [C.ps("pl%d" % i, [128, 4, 128], F32) for i in range(2)]
        S.psum_keys.update([("pq", 0), ("pq", 1), "pss", "pvf", ("po", 0), ("po", 1), ("pl", 0), ("pl", 1)])

        S.dma("pool", "c_id", idb[:], ident, writes=["idb"])
        S.dma("pool", "c_mn", mneg[:], maskneg, writes=["mneg"])
        S.dma("sp", "c_g", gcol[:], g_ap, writes=["gcol"])
        S.dma("sp", "c_gq", gq[:], gq_ap, writes=["gq"])
        S.dma("sp", "c_gk", gk[:], gk_ap, writes=["gk"])
        S.pool(lambda e: e.memset(onesb[:], 1.0), writes=["onesb"])
        wv_all = w_qkv.rearrange("(kc p) n -> p kc n", p=128)

        units = [(h, gi) for h in range(SWA_H) for gi in range(3)]

        def load_w(u):
            h, gi = units[u]
            s = u % 2
            for t, wt, nm in ((0, wq, "wq"), (1, wk, "wk"), (2, wv, "wv")):
                c0 = ((gi * 3 + t) * SWA_H + h) * 128
                S.dma("pool", "%s%d" % (nm, s), wt[s][:], wv_all[:, :, c0:c0 + 128], writes=[(nm, s)])

        load_w(0)
        for j in range(NTL):
            s = j % 2
            S.dma("sp", "xn%d" % s, xn[s][:], x_src[j * 128:(j + 1) * 128, :], writes=[("xn", s)])
            emit_norm_tile2(C, xn[s], ("xn", s), hb[s], ("hb", s), junk, st[s], ("st", s))
            emit_transpose_tile(C, hb[s], ("hb", s), idb, pv, "pvf", hT[:, :, j * 128:(j + 1) * 128], "hT", gcol)

        LN_SCALE = -0.5 * math.log(128.0)
        for u, (h, gi) in enumerate(units):
            s = u % 2
            if u + 1 < len(units):
                load_w(u + 1)
            window, Dg = SWA_PATTERNS[gi]
            L = S_len // Dg
            nb = L // 128
            def proj_mm(ci, wt, nm, ch):
                cs = slice(ch * 512, (ch + 1) * 512)
                i2 = ci % 2
                bank = pq[i2]
                for kc in range(8):
                    S.pe(lambda e, kc=kc, bank=bank, wt=wt, s=s, cs=cs: e.matmul(
                        bank[:], lhsT=wt[s][:, kc, :], rhs=hT[:, kc, cs], start=(kc == 0), stop=(kc == 7)),
                        reads=[(nm, s), "hT"], writes=[("pq", i2)])
                S.act(lambda e, bank=bank, i2=i2: e.activation(out=sq[i2][:], in_=bank[:], func=AF.Square),
                      reads=[("pq", i2)], writes=[("sq", i2)])
                S.dve(lambda e, bank=bank, i2=i2: e.tensor_copy(out=raw[i2][:], in_=bank[:]),
                      reads=[("pq", i2)], writes=[("raw", i2)])

            def proj_norm(ci, dst, dkey, gain, extra, ch):
                cs = slice(ch * 512, (ch + 1) * 512)
                i2 = ci % 2
                S.pe(lambda e, i2=i2: e.matmul(pss[:], lhsT=onesb[:], rhs=sq[i2][:], start=True, stop=True),
                     reads=["onesb", ("sq", i2)], writes=["pss"])
                S.act(lambda e, i2=i2: e.activation(out=rs[i2][:], in_=pss[:], func=AF.Ln, bias=EPS, scale=1.0 / 128.0),
                      reads=["pss"], writes=[("rs", i2)])
                S.act(lambda e, i2=i2, extra=extra: e.activation(out=rs[i2][:], in_=rs[i2][:], func=AF.Exp, scale=-0.5,
                                                                 bias=extra),
                      reads=[("rs", i2)], writes=[("rs", i2)])
                S.dve(lambda e, i2=i2, dst=dst, cs=cs, gain=gain, gi=gi: e.scalar_tensor_tensor(
                    out=dst[:, cs], in0=raw[i2][:], scalar=gain[:, gi:gi + 1], in1=rs[i2][:],
                    op0=ALU.mult, op1=ALU.mult), reads=[("raw", i2), ("rs", i2), "gq", "gk"], writes=[dkey])

            def v_group(b4):
                for bb in range(4):
                    blk = b4 * 4 + bb
                    r, b = divmod(blk, nb)
                    t0 = Dg * 128 * b + r
                    for kc in range(8):
                        S.pe(lambda e, kc=kc, bb=bb, t0=t0, Dg=Dg, s=s: e.matmul(
                            pvf[:, bb, :], lhsT=hT[:, kc, t0:t0 + 127 * Dg + 1:Dg], rhs=wv[s][:, kc, :],
                            start=(kc == 0), stop=(kc == 7)), reads=["hT", ("wv", s)], writes=["pvf"])
                S.act(lambda e, b4=b4: e.copy(out=V[:, b4 * 4:(b4 + 1) * 4, :], in_=pvf[:]),
                      reads=["pvf"], writes=["V"])

            plist = [(wq, "wq", qT, "qT", gq, LN_SCALE, ch) for ch in range(NCH)] + \
                    [(wk, "wk", kT, "kT", gk, 0.0, ch) for ch in range(NCH)]
            nvg = NTL // 4
            vdone = 0
            pend = None
            for ci, (wt, nm, dst, dkey, gain, extra, ch) in enumerate(plist):
                proj_mm(ci, wt, nm, ch)
                if pend is not None:
                    proj_norm(*pend)
                pend = (ci, dst, dkey, gain, extra, ch)
                want = ((ci + 1) * nvg) // len(plist)
                while vdone < want:
                    v_group(vdone)
                    vdone += 1
            proj_norm(*pend)
            while vdone < nvg:
                v_group(vdone)
                vdone += 1

            def scores(pos, r, kb):
                nq = 256 if kb + 1 < nb else 128
                k0 = Dg * 128 * kb + r
                pi = pos % 3
                bi = pos % 2
                bank = pst[bi]
                S.pe(lambda e, bank=bank, k0=k0, Dg=Dg, nq=nq: e.matmul(
                    bank[:, 0:nq], lhsT=kT[:, k0:k0 + 127 * Dg + 1:Dg], rhs=qT[:, k0:k0 + (nq - 1) * Dg + 1:Dg],
                    start=True, stop=False), reads=["kT", "qT"], writes=[("pq", bi)])
                S.pe(lambda e, bank=bank, nq=nq: e.matmul(
                    bank[:, 0:nq], lhsT=idb[:], rhs=mneg[:, 0:nq], start=False, stop=True),
                    reads=["idb", "mneg"], writes=[("pq", bi)])
                S.act(lambda e, bank=bank, pi=pi, nq=nq: e.activation(out=PT[pi][:, 0:nq], in_=bank[:, 0:nq], func=AF.Exp),
                      reads=[("pq", bi)], writes=[("PT", pi)])

            grp = [0]

            def pvacc(pos, r, qb):
                srcs = []
                if qb >= 1:
                    srcs.append((r * nb + qb - 1, (pos - 1) % 3, slice(128, 256)))
                srcs.append((r * nb + qb, pos % 3, slice(0, 128)))
                q4 = qb % 4
                g2 = grp[0] % 2
                for dst_ps, dkey, use_v in ((po[g2], ("po", g2), True), (pl[g2], ("pl", g2), False)):
                    for si, (vblk, pi, csl) in enumerate(srcs):
                        S.pe(lambda e, dst_ps=dst_ps, use_v=use_v, vblk=vblk, pi=pi, csl=csl, q4=q4, si=si, n=len(srcs): e.matmul(
                            dst_ps[:, q4, :], lhsT=(V[:, vblk, :] if use_v else onesb[:]), rhs=PT[pi][:, csl],
                            start=(si == 0), stop=(si == n - 1)),
                            reads=["V", "onesb", ("PT", pi)], writes=[dkey])
                if q4 == 3 or qb == nb - 1:
                    nqb = q4 + 1
                    qb0 = qb - q4
                    t0 = Dg * 128 * qb0 + r
                    tsl = slice(t0, t0 + (nqb * 128 - 1) * Dg + 1, Dg)
                    for dst_ps, dkey, acc, akey in ((po[g2], ("po", g2), oacc, "oacc"), (pl[g2], ("pl", g2), lacc, "lacc")):
                        src = dst_ps[:, 0:nqb, :].rearrange("p a b -> p (a b)")
                        if gi == 0:
                            S.dve(lambda e, src=src, acc=acc, tsl=tsl: e.tensor_copy(out=acc[:, tsl], in_=src),
                                  reads=[dkey], writes=[akey])
                        else:
                            S.dve(lambda e, src=src, acc=acc, tsl=tsl: e.tensor_tensor(
                                out=acc[:, tsl], in0=src, in1=acc[:, tsl], op=ALU.add),
                                reads=[dkey, akey], writes=[akey])
                    grp[0] += 1

            seq = [(r, kb) for r in range(Dg) for kb in range(nb)]
            prev = None
            for pos, (r, kb) in enumerate(seq):
                scores(pos, r, kb)
                if prev is not None:
                    pvacc(*prev)
                prev = (pos, r, kb)
            pvacc(*prev)
            if gi == 2:
                S.act(lambda e: e.activation(out=lacc[:], in_=lacc[:], func=AF.Ln), reads=["lacc"], writes=["lacc"])
                S.act(lambda e: e.activation(out=lacc[:], in_=lacc[:], func=AF.Exp, scale=-1.0), reads=["lacc"], writes=["lacc"])
                S.dve(lambda e: e.tensor_tensor(out=onb[:], in0=oacc[:], in1=lacc[:], op=ALU.mult),
                      reads=["oacc", "lacc"], writes=["onb"])
                S.dma("sp", "ost", onT[h * 128:(h + 1) * 128, :], onb[:], reads=["onb"])
        S.emit(final_chans=["ost"])


def phase_swa_out(nc, x_src, x_dst, onT, w_out, S_len):
    import contextlib
    NTL = S_len // 128
    with contextlib.ExitStack() as es:
        C = Ctx(nc, es)
        S = C.S
        wout_sb = C.sb("wout", [128, 8, D], BF16)
        ot = [C.sb("ot%d" % i, [128, 8, 128], BF16) for i in range(2)]
        xr = [C.sb("xr%d" % i, [128, D], F32) for i in range(2)]
        pp = [C.ps("pp%d" % i, [128, 512], F32) for i in range(4)]
        S.psum_keys.update([("pp", i) for i in range(4)])
        S.dma("pool", "w_out", wout_sb[:], w_out.rearrange("(kc p) n -> p kc n", p=128), writes=["wout"])
        onT_v = onT.rearrange("(h p) t -> p h t", p=128)
        for j in range(NTL):
            s = j % 2
            S.dma("sp", "ot%d" % s, ot[s][:], onT_v[:, :, j * 128:(j + 1) * 128], writes=[("ot", s)])
            S.dma("sp", "xr%d" % s, xr[s][:], x_src[j * 128:(j + 1) * 128, :], writes=[("xr", s)])
            for half in range(2):
                bi = (2 * j + half) % 4
                bank = pp[bi]
                for c8 in range(8):
                    S.pe(lambda e, c8=c8, half=half, bank=bank, s=s: e.matmul(
                        bank[:], lhsT=ot[s][:, c8, :], rhs=wout_sb[:, c8, half * 512:(half + 1) * 512],
                        start=(c8 == 0), stop=(c8 == 7)), reads=[("ot", s), "wout"], writes=[("pp", bi)])
                S.dve(lambda e, s=s, half=half, bank=bank: e.tensor_tensor(
                    out=xr[s][:, half * 512:(half + 1) * 512], in0=bank[:],
                    in1=xr[s][:, half * 512:(half + 1) * 512], op=ALU.add),
                    reads=[("pp", bi), ("xr", s)], writes=[("xr", s)])
            S.dma("pool", "xs%d" % s, x_dst[j * 128:(j + 1) * 128, :], xr[s][:], reads=[("xr", s)])
        S.emit(final_chans=["xs0", "xs1"])


DEPTH = 4


def build_program(S_len):
    nc = bass.Bass("TRN2", target_bir_lowering=False)

    def inp(name, shape):
        return nc.dram_tensor(name, shape, F32, kind="ExternalInput").ap()

    x = inp("x", [S_len, D])
    norm_mix = inp("norm_mix", [4, 128, 8])
    norm_mlp = inp("norm_mlp", [4, 128, 8])
    gla_w_in = inp("gla_w_in", [2, D, GLA_IN])
    gla_w_gate = inp("gla_w_gate_up", [2, 16, 512])
    gla_b_gate = inp("gla_b_gate", [2, 1, 512])
    gla_g_out = inp("gla_g_out", [2, 1, 1024])
    gla_w_out = inp("gla_w_out", [2, D, D])
    swa_w_qkv = inp("swa_w_qkv", [2, D, SWA_IN])
    swa_g_q = inp("swa_g_q", [2, 128, 3])
    swa_g_k = inp("swa_g_k", [2, 128, 3])
    swa_w_out = inp("swa_w_out", [2, D, D])
    mlp_w_up = inp("mlp_w_up", [4, D, DFF])
    mlp_w_down = inp("mlp_w_down", [4, DFF, D])
    ident = inp("c_ident", [128, 128])
    tri = inp("c_tri", [128, 128])
    trineg = inp("c_trineg", [128, 128])
    maskneg = inp("c_maskneg", [128, 256])
    out = nc.dram_tensor("out", [S_len, D], F32, kind="ExternalOutput").ap()
    onT = nc.dram_tensor("onT_scratch", [D, S_len], BF16).ap()
    src = x
    for i in range(DEPTH):
        j = i // 2
        if i % 2 == 0:
            phase_gla(nc, src, out, norm_mix[i], gla_w_in[j], gla_w_gate[j], gla_b_gate[j], gla_g_out[j],
                      gla_w_out[j], ident, tri, trineg, S_len)
        else:
            phase_swa_attn(nc, src, onT, norm_mix[i], swa_w_qkv[j], swa_g_q[j], swa_g_k[j], ident, maskneg, S_len)
            phase_swa_out(nc, src, out, onT, swa_w_out[j], S_len)
        src = out
        phase_mlp(nc, src, out, norm_mlp[i], mlp_w_up[i], mlp_w_down[i], ident, S_len)
    return nc


def make_consts():
    jj, ii = np.meshgrid(np.arange(128), np.arange(128), indexing="ij")
    tri = (jj <= ii).astype(np.float32)
    i2, c2 = np.meshgrid(np.arange(128), np.arange(256), indexing="ij")
    mk = np.where((c2 >= i2) & (c2 <= i2 + 128), 0.0, -30000.0).astype(np.float32)
    return {"c_ident": np.eye(128, dtype=np.float32), "c_tri": tri, "c_trineg": (-tri / 16.0).astype(np.float32),
            "c_maskneg": mk}


def layout_inputs(inputs):
    f = lambda a: np.ascontiguousarray(np.asarray(a, dtype=np.float32))
    shared = {
        "norm_mix": f(np.asarray(inputs["norm_mix"]).reshape(-1, 8, 128).transpose(0, 2, 1)),
        "norm_mlp": f(np.asarray(inputs["norm_mlp"]).reshape(-1, 8, 128).transpose(0, 2, 1)),
        "gla_w_in": f(inputs["gla_w_in"]),
        "gla_w_gate_up": f(inputs["gla_w_gate_up"]),
        "gla_b_gate": f(np.asarray(inputs["gla_b_gate"]).reshape(2, 1, 512)),
        "gla_g_out": f(np.asarray(inputs["gla_g_out"]).reshape(2, 1, 1024)),
        "gla_w_out": f(inputs["gla_w_out"]),
        "swa_w_qkv": f(inputs["swa_w_qkv"]),
        "swa_g_q": f(np.asarray(inputs["swa_g_q"]).transpose(0, 2, 1)),
        "swa_g_k": f(np.asarray(inputs["swa_g_k"]).transpose(0, 2, 1)),
        "swa_w_out": f(inputs["swa_w_out"]),
        "mlp_w_up": f(inputs["mlp_w_up"]),
        "mlp_w_down": f(inputs["mlp_w_down"]),
    }
    shared.update(make_consts())
    return shared


def kernel(**inputs):
    x = np.asarray(inputs["x"], dtype=np.float32)
    B, S_len, _ = x.shape
    shared = layout_inputs(inputs)
    nc = build_program(S_len)
    in_maps = []
    for b in range(B):
        m = dict(shared)
        m["x"] = np.ascontiguousarray(x[b])
        in_maps.append(m)
    res = run_bass_kernel_spmd(nc, in_maps, core_ids=list(range(B)))
    return np.stack([np.asarray(r["out"], dtype=np.float32) for r in res.results], axis=0)
```

```python
import numpy as np
import concourse.bass as bass
import concourse.mybir as mybir
from concourse.bass_utils import run_bass_kernel_spmd

F32 = mybir.dt.float32
BF16 = mybir.dt.bfloat16
AF = mybir.ActivationFunctionType
ALU = mybir.AluOpType
AX = mybir.AxisListType

SAME_ENGINE_SYNC = True


class Sched:
    ENGS = ("pe", "act", "dve", "pool", "sp")
    _semn = [0]

    def __init__(self, nc):
        self.nc = nc
        self.ops = {e: [] for e in self.ENGS}
        self.last_w = {}
        self.readers = {}
        self.chan_cnt = {}
        self.seen = {e: {} for e in self.ENGS}
        self.epoch = 0
        self.needs_inc = set()
        self.psum_keys = set()
        self.last_x = {}

    def new_epoch(self):
        self.epoch += 1

    def add(self, eng, fn, reads=(), writes=(), chan=None):
        idx = len(self.ops[eng])
        deps = []
        for k in reads:
            t = self.last_w.get(k)
            if t is not None:
                deps.append(t)
        for k in writes:
            t = self.last_w.get(k)
            if t is not None and not (chan is not None and t[0] == "d" and t[1] == chan):
                deps.append(t)
            deps.extend(self.readers.get(k, {}).values())
        xkeys = [k for k in tuple(reads) + tuple(writes) if k in self.psum_keys]
        for k in xkeys:
            t = self.last_x.get(k)
            if t is not None and t[1][0] != eng:
                deps.append(t)
        if chan is not None:
            c = self.chan_cnt.get(chan, 0) + 1
            self.chan_cnt[chan] = c
            tok = ("d", chan, c)
        else:
            tok = ("c", (eng, self.epoch), idx)
        best = {}
        for t in deps:
            kind, sk, v = t
            if kind == "c":
                if sk[0] == eng and (eng == "pe" or not SAME_ENGINE_SYNC):
                    continue
                if sk[0] == eng and v >= idx:
                    continue
            key = (kind, sk)
            if best.get(key, -1) < v:
                best[key] = v
        waits = []
        seen = self.seen[eng]
        for key, v in best.items():
            if seen.get(key, -1) >= v:
                continue
            seen[key] = v
            waits.append((key[0], key[1], v))
            if key[0] == "c":
                self.needs_inc.add((key[1], v))
        self.ops[eng].append(dict(fn=fn, waits=waits, chan=chan, epoch=self.epoch))
        for k in xkeys:
            self.last_x[k] = tok
        for k in writes:
            self.last_w[k] = tok
            self.readers[k] = {}
        for k in reads:
            self.readers.setdefault(k, {})[(tok[0], tok[1])] = tok
        return tok

    def pe(self, fn, reads=(), writes=()):
        return self.add("pe", fn, reads, writes)

    def act(self, fn, reads=(), writes=()):
        return self.add("act", fn, reads, writes)

    def dve(self, fn, reads=(), writes=()):
        return self.add("dve", fn, reads, writes)

    def pool(self, fn, reads=(), writes=()):
        return self.add("pool", fn, reads, writes)

    def dma(self, queue, chan, out, in_, reads=(), writes=()):
        return self.add(queue, lambda e: e.dma_start(out=out, in_=in_), reads, writes, chan=chan)

    def emit(self, final_chans=()):
        nc = self.nc
        last_c = {}
        for e in self.ENGS:
            for idx in range(len(self.ops[e]) - 1, -1, -1):
                if self.ops[e][idx]["chan"] is None:
                    sk = (e, self.ops[e][idx]["epoch"])
                    self.needs_inc.add((sk, idx))
                    last_c[e] = (sk, idx)
                    break
        cnt = {}
        incval = {}
        for e in self.ENGS:
            for idx, op in enumerate(self.ops[e]):
                sk = (e, op["epoch"])
                if (sk, idx) in self.needs_inc:
                    cnt[sk] = cnt.get(sk, 0) + 1
                    incval[(sk, idx)] = cnt[sk]
        sem_keys = sorted(cnt.keys()) + sorted(("chan", c) for c in self.chan_cnt)
        sems = {}
        for k in sem_keys:
            Sched._semn[0] += 1
            sems[k] = nc.alloc_semaphore("sem%d" % Sched._semn[0])
        with nc.Block() as block:

            def run(ename, eng):
                for idx, op in enumerate(self.ops[ename]):
                    for kind, sk, v in op["waits"]:
                        if kind == "c":
                            eng.wait_ge(sems[sk], incval[(sk, v)])
                        else:
                            eng.wait_ge(sems[("chan", sk)], 16 * v)
                    ins = op["fn"](eng)
                    sk = (ename, op["epoch"])
                    if op["chan"] is not None:
                        ins.then_inc(sems[("chan", op["chan"])], 16)
                    elif (sk, idx) in incval:
                        ins.then_inc(sems[sk], 1)
                for e2, (sk, idx) in last_c.items():
                    if e2 != ename:
                        eng.wait_ge(sems[sk], incval[(sk, idx)])
                for c, n in self.chan_cnt.items():
                    eng.wait_ge(sems[("chan", c)], 16 * n)

            @block.tensor
            def _(e):
                run("pe", e)

            @block.scalar
            def _(e):
                run("act", e)

            @block.vector
            def _(e):
                run("dve", e)

            @block.gpsimd
            def _(e):
                run("pool", e)

            @block.sync
            def _(e):
                run("sp", e)
        nc.all_engine_barrier()
        nc.clear_and_free_semaphores(list(sems.values()))
        nc.all_engine_barrier()

    def barrier_deps(self):
        return None


D = 1024
DFF = 4096
EPS = 1e-6
NT = 128


class Ctx:
    _n = [0]

    def __init__(self, nc, es):
        self.nc = nc
        self.es = es
        self.S = Sched(nc)
        Ctx._n[0] += 1
        self.tag = "p%d_" % Ctx._n[0]

    def sb(self, name, shape, dt):
        return self.es.enter_context(self.nc.sbuf_tensor(self.tag + name, shape, dt))

    def ps(self, name, shape, dt):
        return self.es.enter_context(self.nc.psum_tensor(self.tag + name, shape, dt))


def emit_norm_tile(C, xin, xkey, hb, hbkey, junk, st, stkey):
    S = C.S
    S.pool(lambda e: e.memset(st[:, 0:1], 0.0), writes=[stkey])
    S.act(lambda e: e.activation(out=junk[:], in_=xin[:], func=AF.Square, scale=1.0 / 32.0,
                                 accum_out=st[:, 0:1]), reads=[xkey, stkey], writes=["junk", stkey])
    S.act(lambda e: e.activation(out=st[:, 1:2], in_=st[:, 0:1], func=AF.Sqrt, bias=EPS, scale=1.0),
          reads=[stkey], writes=[stkey])
    S.dve(lambda e: e.reciprocal(out=st[:, 2:3], in_=st[:, 1:2]), reads=[stkey], writes=[stkey])
    S.act(lambda e: e.activation(out=hb[:], in_=xin[:], func=AF.Copy, scale=st[:, 2:3]),
          reads=[xkey, stkey], writes=[hbkey])


def emit_transpose_tile(C, hb, hbkey, idb, ptr, ptrkey, hT_dst, hTkey, gcol):
    S = C.S
    for c in range(8):
        S.pe(lambda e, c=c: e.transpose(out=ptr[:, c, :], in_=hb[:, c * 128:(c + 1) * 128], identity=idb[:]),
             reads=[hbkey, "idb"], writes=[ptrkey])
    S.dve(lambda e: e.tensor_tensor(out=hT_dst, in0=ptr[:, :, :],
                                    in1=gcol[:, :].unsqueeze(2).to_broadcast([128, 8, 128]), op=ALU.mult),
          reads=[ptrkey, "gcol"], writes=[hTkey])


def phase_mlp(nc, x_src, x_dst, g_ap, wup, wdn, ident, S_len):
    import contextlib
    M = S_len // 512
    with contextlib.ExitStack() as es:
        C = Ctx(nc, es)
        S = C.S
        wup_sb = C.sb("wup", [128, 8, DFF], BF16)
        wdn_sb = C.sb("wdn", [128, 32, D], BF16)
        idb = C.sb("idb", [128, 128], BF16)
        gcol = C.sb("gcol", [128, 8], F32)
        xn = [C.sb("xn%d" % i, [128, D], F32) for i in range(2)]
        hb = [C.sb("hb%d" % i, [128, D], BF16) for i in range(4)]
        st = [C.sb("st%d" % i, [128, 4], F32) for i in range(2)]
        junk = C.sb("junk", [128, D], BF16)
        hT = [C.sb("hT%d" % i, [128, 8, 512], BF16) for i in range(2)]
        actT = C.sb("actT", [128, 32, 512], BF16)
        rl = [C.sb("rl%d" % i, [128, 512], F32) for i in range(2)]
        xr = [C.sb("xr%d" % i, [128, D], F32) for i in range(2)]
        pu = [C.ps("pu%d" % i, [128, 512], F32) for i in range(3)]
        pd = [C.ps("pd%d" % i, [128, 512], F32) for i in range(4)]
        ptr = C.ps("ptr", [128, 8, 128], BF16)
        S.psum_keys.update([("pu", i) for i in range(3)] + [("pd", i) for i in range(4)] + ["ptr"])

        S.dma("pool", "c_id", idb[:], ident, writes=["idb"])
        S.dma("sp", "c_g", gcol[:], g_ap, writes=["gcol"])
        wup_v = wup.rearrange("(kc p) n -> p kc n", p=128)
        wdn_v = wdn.rearrange("(fc p) n -> p fc n", p=128)
        for g in range(8):
            S.dma("pool", "w_up%d" % g, wup_sb[:, :, g * 512:(g + 1) * 512], wup_v[:, :, g * 512:(g + 1) * 512],
                  writes=[("wup", g)])
        for g in range(8):
            S.dma("pool", "w_dn%d" % g, wdn_sb[:, g * 4:(g + 1) * 4, :], wdn_v[:, g * 4:(g + 1) * 4, :],
                  writes=[("wdn", g)])

        def load_xn(j):
            s = j % 2
            S.dma("sp", "xn%d" % s, xn[s][:], x_src[j * 128:(j + 1) * 128, :], writes=[("xn", s)])

        def norm_elem(j):
            s = j % 2
            emit_norm_tile(C, xn[s], ("xn", s), hb[j % 4], ("hb", j % 4), junk, st[s], ("st", s))

        def transp(j):
            m, q = divmod(j, 4)
            emit_transpose_tile(C, hb[q], ("hb", q), idb, ptr, "ptr",
                                hT[m % 2][:, :, q * 128:(q + 1) * 128], ("hT", m % 2), gcol)

        for q in range(4):
            load_xn(q)
            norm_elem(q)
            transp(q)
        for m in range(M):
            ms = m % 2
            for g in range(32):
                bank = pu[g % 3]
                for kc in range(8):
                    S.pe(lambda e, g=g, kc=kc, bank=bank, ms=ms: e.matmul(
                        bank[:], lhsT=wup_sb[:, kc, g * 128:(g + 1) * 128], rhs=hT[ms][:, kc, :],
                        start=(kc == 0), stop=(kc == 7)),
                        reads=[("wup", g // 4), ("hT", ms)], writes=[("pu", g % 3)])
                r = rl[g % 2]
                S.act(lambda e, bank=bank, r=r: e.activation(out=r[:], in_=bank[:], func=AF.Relu),
                      reads=[("pu", g % 3)], writes=[("rl", g % 2)])
                S.dve(lambda e, g=g, r=r: e.tensor_tensor(out=actT[:, g, :], in0=r[:], in1=r[:], op=ALU.mult),
                      reads=[("rl", g % 2)], writes=["actT"])
                if m + 1 < M and g % 8 == 1:
                    q = g // 8
                    load_xn((m + 1) * 4 + q)
                    norm_elem((m + 1) * 4 + q)
            if m + 1 < M:
                for q in range(4):
                    transp((m + 1) * 4 + q)
            for q in range(4):
                j = m * 4 + q
                s = j % 2
                S.dma("pool", "xr%d" % s, xr[s][:], x_src[j * 128:(j + 1) * 128, :], writes=[("xr", s)])
                for half in range(2):
                    bank = pd[(2 * q + half) % 4]
                    bk = ("pd", (2 * q + half) % 4)
                    for fc in range(32):
                        S.pe(lambda e, fc=fc, q=q, half=half, bank=bank: e.matmul(
                            bank[:], lhsT=actT[:, fc, q * 128:(q + 1) * 128],
                            rhs=wdn_sb[:, fc, half * 512:(half + 1) * 512],
                            start=(fc == 0), stop=(fc == 31)),
                            reads=["actT", ("wdn", fc // 4)], writes=[bk])
                    S.dve(lambda e, s=s, half=half, bank=bank: e.tensor_tensor(
                        out=xr[s][:, half * 512:(half + 1) * 512], in0=bank[:],
                        in1=xr[s][:, half * 512:(half + 1) * 512], op=ALU.add),
                        reads=[bk, ("xr", s)], writes=[("xr", s)])
                S.dma("pool", "xs%d" % s, x_dst[j * 128:(j + 1) * 128, :], xr[s][:], reads=[("xr", s)])
        S.emit(final_chans=["xs0", "xs1"])


GLA_H = 4
STAGE = 99
GLA_IN = 3088


def emit_rstd(C, st, stkey, n):
    S = C.S
    S.act(lambda e: e.activation(out=st[:, n:2 * n], in_=st[:, 0:n], func=AF.Ln, bias=EPS, scale=1.0),
          reads=[stkey], writes=[stkey])
    S.act(lambda e: e.activation(out=st[:, 2 * n:3 * n], in_=st[:, n:2 * n], func=AF.Exp, scale=-0.5),
          reads=[stkey], writes=[stkey])


def emit_norm_tile2(C, xin, xkey, hb, hbkey, junk, st, stkey):
    S = C.S
    S.pool(lambda e: e.memset(st[:, 0:1], 0.0), writes=[stkey])
    S.act(lambda e: e.activation(out=junk[:], in_=xin[:], func=AF.Square, scale=1.0 / 32.0,
                                 accum_out=st[:, 0:1]), reads=[xkey, stkey], writes=["junk", stkey])
    emit_rstd(C, st, stkey, 1)
    S.act(lambda e: e.activation(out=hb[:], in_=xin[:], func=AF.Copy, scale=st[:, 2:3]),
          reads=[xkey, stkey], writes=[hbkey])


def phase_gla(nc, x_src, x_dst, g_ap, w_in, w_gate, b_gate, g_out, w_out, ident, tri, trineg, S_len):
    import contextlib
    M = S_len // 512
    with contextlib.ExitStack() as es:
        C = Ctx(nc, es)
        S = C.S
        win_sb = C.sb("win", [128, 8, GLA_IN], BF16)
        wout_sb = C.sb("wout", [128, 8, D], BF16)
        idb = C.sb("idb", [128, 128], BF16)
        gcol = C.sb("gcol", [128, 8], F32)
        trim = C.sb("trim", [128, 128], F32)
        trin = C.sb("trin", [128, 128], BF16)
        wg_sb = C.sb("wg", [128, 512], F32)
        wg_hi = C.sb("wg_hi", [128, 512], BF16)
        wg_lo = C.sb("wg_lo", [128, 512], BF16)
        zT_hi = C.sb("zT_hi", [128, 512], BF16)
        zT_lo = C.sb("zT_lo", [128, 512], BF16)
        goutB = C.sb("goutB", [128, D], F32)
        xn = [C.sb("xn%d" % i, [128, D], F32) for i in range(2)]
        hb = [C.sb("hb0", [128, D], BF16)]
        st = [C.sb("st%d" % i, [128, 4], F32) for i in range(2)]
        junk = C.sb("junk", [128, D], BF16)
        hT = C.sb("hT", [128, 8, 512], BF16)
        qTf = C.sb("qTf", [128, 4, 512], F32)
        kTf = C.sb("kTf", [128, 4, 512], F32)
        zT = C.sb("zT", [128, 512], F32)
        v_sb = C.sb("v", [128, 4, D], BF16)
        r_sb = C.sb("r", [128, D], F32)
        sr = C.sb("sr", [128, 4, D], F32)
        et = C.sb("et", [128, D], F32)
        glf = C.sb("glf", [128, 512], F32)
        gl_hi = C.sb("gl_hi", [128, 4, 512], BF16)
        gl_lo = C.sb("gl_lo", [128, 4, 512], BF16)
        eb = [C.sb("eb%d" % i, [128, 4, 128], F32) for i in range(2)]
        enb = C.sb("enb", [128, 4, 128], F32)
        qt = [C.sb("qt%d" % i, [128, 4, 128], BF16) for i in range(2)]
        kt = [C.sb("kt%d" % i, [128, 4, 128], BF16) for i in range(2)]
        ktok = C.sb("ktok", [128, 4, 128], BF16)
        AT = C.sb("AT", [128, 4, 128], BF16)
        St = C.sb("St", [128, 4, 256], F32)
        Sbf = [C.sb("Sbf%d" % i, [128, 4, 256], BF16) for i in range(2)]
        ost = C.sb("ost", [128, 12], F32)
        on = C.sb("on", [128, 4, 256], F32)
        gated = C.sb("gated", [128, D], BF16)
        gT = C.sb("gT", [128, 8, 128], BF16)
        xr = [C.sb("xr%d" % i, [128, D], F32) for i in range(2)]
        pp = [C.ps("pp%d" % i, [128, 512], F32) for i in range(2)]
        ptr = C.ps("ptr", [128, 8, 128], BF16)
        pba = C.ps("pba", [128, 4, 128], F32)
        po = C.ps("po", [128, 4, 256], F32)
        pS = C.ps("pS", [128, 4, 256], F32)
        S.psum_keys.update([("pp", 0), ("pp", 1), "ptr", "pba", "po", "pS"])

        S.dma("pool", "c_id", idb[:], ident, writes=["idb"])
        S.dma("sp", "c_g", gcol[:], g_ap, writes=["gcol"])
        S.dma("sp", "c_t1", trim[:], tri, writes=["trim"])
        S.dma("pool", "c_t2", trin[:], trineg, writes=["trin"])
        S.pool(lambda e: e.memset(wg_sb[:], 0.0), writes=["wg"])
        S.dma("sp", "c_wg", wg_sb[0:16, :], w_gate, writes=["wg"])
        S.dma("sp", "c_wg", wg_sb[16:17, :], b_gate, writes=["wg"])
        S.dve(lambda e: e.tensor_copy(out=wg_hi[:], in_=wg_sb[:]), reads=["wg"], writes=["wg_hi"])
        S.dve(lambda e: e.tensor_tensor(out=wg_lo[:], in0=wg_sb[:], in1=wg_hi[:], op=ALU.subtract),
              reads=["wg", "wg_hi"], writes=["wg_lo"])
        S.pool(lambda e: e.memset(zT[:], 0.0), writes=["zT"])
        S.pool(lambda e: e.memset(zT[0:32, :], 1.0), writes=["zT"])
        S.dma("sp", "c_go", goutB[:], g_out.partition_broadcast(128), writes=["goutB"])
        S.pool(lambda e: e.memset(St[:], 0.0), writes=["St"])
        S.pool(lambda e: e.memset(Sbf[0][:], 0.0), writes=[("Sbf", 0)])
        win_v = w_in.rearrange("(kc p) n -> p kc n", p=128)
        wout_v = w_out.rearrange("(kc p) n -> p kc n", p=128)
        blocks = [(0, 1024), (1024, 2048), (2048, 3072), (3072, 3088)]
        for bi, (c0, c1) in enumerate(blocks):
            S.dma("pool", "w_in%d" % bi, win_sb[:, :, c0:c1], win_v[:, :, c0:c1], writes=[("win", bi)])
        S.dma("pool", "w_out", wout_sb[:], wout_v, writes=["wout"])

        pcnt = [0]

        def pbank():
            i = pcnt[0] % 2
            pcnt[0] += 1
            return pp[i], ("pp", i)

        def P0(m):
            for q in range(4):
                j = m * 4 + q
                s = j % 2
                S.dma("sp", "xn%d" % s, xn[s][:], x_src[j * 128:(j + 1) * 128, :], writes=[("xn", s)])
                emit_norm_tile2(C, xn[s], ("xn", s), hb[0], ("hb", 0), junk, st[s], ("st", s))
                emit_transpose_tile(C, hb[0], ("hb", 0), idb, ptr, "ptr",
                                    hT[:, :, q * 128:(q + 1) * 128], "hT", gcol)
            bank, bk = pbank()
            for kc in range(8):
                S.pe(lambda e, kc=kc, bank=bank: e.matmul(
                    bank[0:16, :], lhsT=win_sb[:, kc, 3072:3088], rhs=hT[:, kc, :],
                    start=(kc == 0), stop=(kc == 7)), reads=[("win", 3), "hT"], writes=[bk])
            S.dve(lambda e, bank=bank: e.tensor_copy(out=zT[0:16, :], in_=bank[0:16, :]), reads=[bk], writes=["zT"])
            S.dve(lambda e: e.tensor_copy(out=zT_hi[:], in_=zT[:]), reads=["zT"], writes=["zT_hi"])
            S.dve(lambda e: e.tensor_tensor(out=zT_lo[:], in0=zT[:], in1=zT_hi[:], op=ALU.subtract),
                  reads=["zT", "zT_hi"], writes=["zT_lo"])

        def Pqk(m):
            for gi in range(8):
                bank, bk = pbank()
                for kc in range(8):
                    S.pe(lambda e, gi=gi, kc=kc, bank=bank: e.matmul(
                        bank[:], lhsT=win_sb[:, kc, gi * 128:(gi + 1) * 128], rhs=hT[:, kc, :],
                        start=(kc == 0), stop=(kc == 7)), reads=[("win", 0), "hT"], writes=[bk])
                if gi < 4:
                    S.act(lambda e, gi=gi, bank=bank: e.activation(out=qTf[:, gi, :], in_=bank[:], func=AF.Copy,
                                                                   scale=128.0 ** -0.5),
                          reads=[bk], writes=["qTf"])
                else:
                    S.dve(lambda e, gi=gi, bank=bank: e.tensor_copy(out=kTf[:, gi - 4, :], in_=bank[:]),
                          reads=[bk], writes=["kTf"])

        def Pv(m, q):
            for half in range(2):
                bank, bk = pbank()
                for kc in range(8):
                    S.pe(lambda e, q=q, half=half, kc=kc, bank=bank: e.matmul(
                        bank[:], lhsT=hT[:, kc, q * 128:(q + 1) * 128],
                        rhs=win_sb[:, kc, 1024 + half * 512:1024 + (half + 1) * 512],
                        start=(kc == 0), stop=(kc == 7)), reads=[("win", 1), "hT"], writes=[bk])
                S.dve(lambda e, q=q, half=half, bank=bank: e.tensor_copy(
                    out=v_sb[:, q, half * 512:(half + 1) * 512], in_=bank[:]), reads=[bk], writes=[("v", q)])
            for half in range(2):
                bank, bk = pbank()
                for kc in range(8):
                    S.pe(lambda e, q=q, half=half, kc=kc, bank=bank: e.matmul(
                        bank[:], lhsT=hT[:, kc, q * 128:(q + 1) * 128],
                        rhs=win_sb[:, kc, 2048 + half * 512:2048 + (half + 1) * 512],
                        start=(kc == 0), stop=(kc == 7)), reads=[("win", 2), "hT"], writes=[bk])
                hs = slice(half * 512, (half + 1) * 512)
                S.act(lambda e, hs=hs, bank=bank: e.activation(out=et[:, hs], in_=bank[:], func=AF.Exp, scale=-1.0),
                      reads=[bk], writes=["et"])
                S.dve(lambda e, hs=hs, bank=bank: e.tensor_copy(out=r_sb[:, hs], in_=bank[:]),
                      reads=[bk], writes=["r"])
            S.act(lambda e: e.activation(out=et[:], in_=et[:], func=AF.Ln, bias=1.0, scale=1.0),
                  reads=["et"], writes=["et"])
            S.act(lambda e, q=q: e.activation(out=sr[:, q, :], in_=et[:], func=AF.Exp, scale=-1.0),
                  reads=["et"], writes=[("sr", q)])
            S.dve(lambda e, q=q: e.tensor_tensor(out=sr[:, q, :], in0=sr[:, q, :], in1=r_sb[:, :], op=ALU.mult),
                   reads=[("sr", q), "r"], writes=[("sr", q)])
            S.dve(lambda e, q=q: e.tensor_tensor(out=sr[:, q, :], in0=sr[:, q, :], in1=goutB[:], op=ALU.mult),
                   reads=[("sr", q), "goutB"], writes=[("sr", q)])
            bank, bk = pbank()
            qs = slice(q * 128, (q + 1) * 128)
            for pi, (za, wa) in enumerate(((zT_hi, wg_hi), (zT_lo, wg_hi), (zT_hi, wg_lo))):
                S.pe(lambda e, qs=qs, bank=bank, za=za, wa=wa, pi=pi: e.matmul(
                    bank[:], lhsT=za[:, qs], rhs=wa[:, :], start=(pi == 0), stop=(pi == 2)),
                    reads=["zT_hi", "zT_lo", "wg_hi", "wg_lo"], writes=[bk])
            S.act(lambda e, bank=bank: e.activation(out=glf[:], in_=bank[:], func=AF.Exp, scale=-1.0),
                  reads=[bk], writes=["glf"])
            S.act(lambda e: e.activation(out=glf[:], in_=glf[:], func=AF.Ln, bias=1.0, scale=1.0),
                  reads=["glf"], writes=["glf"])
            S.dve(lambda e, q=q: e.tensor_copy(out=gl_hi[:, q, :], in_=glf[:]), reads=["glf"], writes=[("gl", q)])
            S.dve(lambda e, q=q: e.tensor_tensor(out=gl_lo[:, q, :], in0=glf[:], in1=gl_hi[:, q, :], op=ALU.subtract),
                  reads=["glf", ("gl", q)], writes=[("gl", q)])

        def Aa(m, q):
            e2 = q % 2
            ts = slice(q * 128, (q + 1) * 128)
            for h in range(4):
                for pi, ga in enumerate((gl_hi, gl_lo)):
                    S.pe(lambda e, q=q, h=h, ga=ga, pi=pi: e.matmul(
                        pba[:, h, :], lhsT=ga[:, q, h * 128:(h + 1) * 128], rhs=trin[:, :],
                        start=(pi == 0), stop=(pi == 1)), reads=[("gl", q), "trin"], writes=["pba"])
            S.act(lambda e, e2=e2: e.activation(out=eb[e2][:], in_=pba[:], func=AF.Exp), reads=["pba"], writes=[("eb", e2)])
            S.act(lambda e: e.activation(out=enb[:], in_=pba[:], func=AF.Exp, scale=-1.0), reads=["pba"], writes=["enb"])
            S.dve(lambda e, ts=ts, e2=e2: e.tensor_tensor(out=qt[e2][:], in0=qTf[:, :, ts], in1=eb[e2][:], op=ALU.mult),
                  reads=["qTf", ("eb", e2)], writes=[("qt", e2)])
            S.dve(lambda e, ts=ts, e2=e2: e.tensor_tensor(out=kt[e2][:], in0=kTf[:, :, ts], in1=enb[:], op=ALU.mult),
                  reads=["kTf", "enb"], writes=[("kt", e2)])

        def Ab(m, q):
            e2 = q % 2
            for h in range(4):
                S.pe(lambda e, h=h, e2=e2: e.transpose(out=ptr[:, h, :], in_=kt[e2][:, h, :], identity=idb[:]),
                     reads=[("kt", e2), "idb"], writes=["ptr"])
            S.act(lambda e: e.copy(out=ktok[:], in_=ptr[:, 0:4, :]), reads=["ptr"], writes=["ktok"])
            for h in range(4):
                S.pe(lambda e, h=h, e2=e2: e.matmul(pba[:, h, :], lhsT=kt[e2][:, h, :], rhs=qt[e2][:, h, :],
                                                    start=True, stop=True),
                     reads=[("kt", e2), ("qt", e2)], writes=["pba"])
            S.dve(lambda e: e.tensor_tensor(out=AT[:], in0=pba[:],
                                            in1=trim[:, :].unsqueeze(1).to_broadcast([128, 4, 128]), op=ALU.mult),
                  reads=["pba", "trim"], writes=["AT"])

        def Bs(m, q):
            e2 = q % 2
            c = m * 4 + q
            sb_in = Sbf[c % 2]
            so = Sbf[(c + 1) % 2]
            for h in range(4):
                S.pe(lambda e, q=q, h=h: e.matmul(po[:, h, :], lhsT=AT[:, h, :], rhs=v_sb[:, q, h * 256:(h + 1) * 256],
                                                  start=(h % 2 == 0), stop=False, skip_group_check=True),
                     reads=["AT", ("v", q)], writes=["po"])
            for h in range(4):
                S.pe(lambda e, h=h, e2=e2, sb_in=sb_in: e.matmul(
                    po[:, h, :], lhsT=qt[e2][:, h, :], rhs=sb_in[:, h, :], start=False, stop=True,
                    skip_group_check=True), reads=[("qt", e2), ("Sbf", c % 2)], writes=["po"])
            for h in range(4):
                S.pe(lambda e, q=q, h=h: e.matmul(
                    pS[:, h, :], lhsT=ktok[:, h, :], rhs=v_sb[:, q, h * 256:(h + 1) * 256],
                    start=True, stop=True), reads=["ktok", ("v", q)], writes=["pS"])
            S.dve(lambda e: e.tensor_tensor(out=St[:], in0=pS[:], in1=St[:], op=ALU.add),
                  reads=["pS", "St"], writes=["St"])
            S.dve(lambda e, e2=e2: e.tensor_tensor(
                out=St[:], in0=St[:], in1=eb[e2][:, :, 127:128].to_broadcast([128, 4, 256]), op=ALU.mult),
                reads=["St", ("eb", e2)], writes=["St"])
            S.act(lambda e, so=so: e.copy(out=so[:], in_=St[:]), reads=["St"], writes=[("Sbf", (c + 1) % 2)])

        def Ca(m, q):
            S.pool(lambda e: e.memset(ost[:, 0:4], 0.0), writes=["ost"])
            for h in range(4):
                S.act(lambda e, h=h: e.activation(out=junk[:, 0:256], in_=po[:, h, :], func=AF.Square, scale=1.0 / 16.0,
                                                  accum_out=ost[:, h:h + 1]), reads=["po", "ost"], writes=["junk", "ost"])
            emit_rstd(C, ost, "ost", 4)
            S.dve(lambda e: e.tensor_tensor(out=on[:], in0=po[:], in1=ost[:, 8:12].unsqueeze(2).to_broadcast([128, 4, 256]),
                                            op=ALU.mult), reads=["po", "ost"], writes=["on"])
            S.dve(lambda e, q=q: e.tensor_tensor(out=gated[:], in0=on[:].rearrange("p h v -> p (h v)"), in1=sr[:, q, :],
                                                  op=ALU.mult), reads=["on", ("sr", q)], writes=["gated"])

        def Cb(m, q):
            for c8 in range(8):
                S.pe(lambda e, c8=c8: e.transpose(out=ptr[:, c8, :], in_=gated[:, c8 * 128:(c8 + 1) * 128], identity=idb[:]),
                     reads=["gated", "idb"], writes=["ptr"])
            S.act(lambda e: e.copy(out=gT[:], in_=ptr[:]), reads=["ptr"], writes=["gT"])

        def Cc(m, q):
            j = m * 4 + q
            s = j % 2
            S.dma("pool", "xr%d" % s, xr[s][:], x_src[j * 128:(j + 1) * 128, :], writes=[("xr", s)])
            for half in range(2):
                bank, bk = pbank()
                for c8 in range(8):
                    S.pe(lambda e, c8=c8, half=half, bank=bank: e.matmul(
                        bank[:], lhsT=gT[:, c8, :], rhs=wout_sb[:, c8, half * 512:(half + 1) * 512],
                        start=(c8 == 0), stop=(c8 == 7)), reads=["gT", "wout"], writes=[bk])
                S.dve(lambda e, s=s, half=half, bank=bank: e.tensor_tensor(
                    out=xr[s][:, half * 512:(half + 1) * 512], in0=bank[:],
                    in1=xr[s][:, half * 512:(half + 1) * 512], op=ALU.add),
                    reads=[bk, ("xr", s)], writes=[("xr", s)])
            S.dma("pool", "xs%d" % s, x_dst[j * 128:(j + 1) * 128, :], xr[s][:], reads=[("xr", s)])


        P0(0)
        Pqk(0)
        for q in range(4):
            Pv(0, q)
        for m in range(M):
            nxt = m + 1 < M
            for n in range(4 + 3):
                if 0 <= n - 1 < 4:
                    Ab(m, n - 1)
                if 0 <= n - 3 < 4:
                    Cc(m, n - 3)
                if 0 <= n - 2 < 4:
                    Cb(m, n - 2)
                if 0 <= n - 1 < 4:
                    Bs(m, n - 1)
                    Ca(m, n - 1)
                if n < 4:
                    Aa(m, n)
                if nxt:
                    if n == 1:
                        P0(m + 1)
                    if n == 3:
                        Pqk(m + 1)
                    if 2 <= n <= 5:
                        Pv(m + 1, n - 2)

        S.emit(final_chans=["xs0", "xs1"])


SWA_PATTERNS = ((128, 1), (512, 4), (2048, 16))
SWA_H = 8
SWA_IN = 9216


def phase_swa_attn(nc, x_src, onT, g_ap, w_qkv, gq_ap, gk_ap, ident, maskneg, S_len):
    import contextlib
    import math
    NTL = S_len // 128
    NCH = S_len // 512
    with contextlib.ExitStack() as es:
        C = Ctx(nc, es)
        S = C.S
        hT = C.sb("hT", [128, 8, S_len], BF16)
        idb = C.sb("idb", [128, 128], BF16)
        onesb = C.sb("onesb", [128, 128], BF16)
        mneg = C.sb("mneg", [128, 256], BF16)
        gcol = C.sb("gcol", [128, 8], F32)
        gq = C.sb("gq", [128, 3], F32)
        gk = C.sb("gk", [128, 3], F32)
        xn = [C.sb("xn%d" % i, [128, D], F32) for i in range(2)]
        hb = [C.sb("hb%d" % i, [128, D], BF16) for i in range(2)]
        st = [C.sb("st%d" % i, [128, 4], F32) for i in range(2)]
        junk = C.sb("junk", [128, D], BF16)
        wq = [C.sb("wq%d" % i, [128, 8, 128], BF16) for i in range(2)]
        wk = [C.sb("wk%d" % i, [128, 8, 128], BF16) for i in range(2)]
        wv = [C.sb("wv%d" % i, [128, 8, 128], BF16) for i in range(2)]
        qT = C.sb("qT", [128, S_len], BF16)
        kT = C.sb("kT", [128, S_len], BF16)
        V = C.sb("V", [128, NTL, 128], BF16)
        raw = [C.sb("raw%d" % i, [128, 512], F32) for i in range(2)]
        sq = [C.sb("sq%d" % i, [128, 512], BF16) for i in range(2)]
        rs = [C.sb("rs%d" % i, [128, 512], F32) for i in range(2)]
        PT = [C.sb("PT%d" % i, [128, 256], BF16) for i in range(3)]
        oacc = C.sb("oacc", [128, S_len], F32)
        lacc = C.sb("lacc", [128, S_len], F32)
        onb = C.sb("onb", [128, S_len], BF16)
        pq = [C.ps("pq%d" % i, [128, 512], F32) for i in range(2)]
        pst = pq
        pss = C.ps("pss", [128, 512], F32)
        pvf = C.ps("pvf", [128, 4, 128], F32)
        pv = pvf[:].bitcast(BF16).rearrange("p a (b c) -> p (a b) c", c=128)
        po = [C.ps("po%d" % i, [128, 4, 128], F32) for i in range(2)]
        pl = [C.ps("pl%d" % i, [128, 4, 128], F32) for i in range(2)]
        S.psum_keys.update([("pq", 0), ("pq", 1), "pss", "pvf", ("po", 0), ("po", 1), ("pl", 0), ("pl", 1)])

        S.dma("pool", "c_id", idb[:], ident, writes=["idb"])
        S.dma("pool", "c_mn", mneg[:], maskneg, writes=["mneg"])
        S.dma("sp", "c_g", gcol[:], g_ap, writes=["gcol"])
        S.dma("sp", "c_gq", gq[:], gq_ap, writes=["gq"])
        S.dma("sp", "c_gk", gk[:], gk_ap, writes=["gk"])
        S.pool(lambda e: e.memset(onesb[:], 1.0), writes=["onesb"])
        wv_all = w_qkv.rearrange("(kc p) n -> p kc n", p=128)

        units = [(h, gi) for h in range(SWA_H) for gi in range(3)]

        def load_w(u):
            h, gi = units[u]
            s = u % 2
            for t, wt, nm in ((0, wq, "wq"), (1, wk, "wk"), (2, wv, "wv")):
                c0 = ((gi * 3 + t) * SWA_H + h) * 128
                S.dma("pool", "%s%d" % (nm, s), wt[s][:], wv_all[:, :, c0:c0 + 128], writes=[(nm, s)])

        load_w(0)
        for j in range(NTL):
            s = j % 2
            S.dma("sp", "xn%d" % s, xn[s][:], x_src[j * 128:(j + 1) * 128, :], writes=[("xn", s)])
            emit_norm_tile2(C, xn[s], ("xn", s), hb[s], ("hb", s), junk, st[s], ("st", s))
            emit_transpose_tile(C, hb[s], ("hb", s), idb, pv, "pvf", hT[:, :, j * 128:(j + 1) * 128], "hT", gcol)

        LN_SCALE = -0.5 * math.log(128.0)
        for u, (h, gi) in enumerate(units):
            s = u % 2
            if u + 1 < len(units):
                load_w(u + 1)
            window, Dg = SWA_PATTERNS[gi]
            L = S_len // Dg
            nb = L // 128
            def proj_mm(ci, wt, nm, ch):
                cs = slice(ch * 512, (ch + 1) * 512)
                i2 = ci % 2
                bank = pq[i2]
                for kc in range(8):
                    S.pe(lambda e, kc=kc, bank=bank, wt=wt, s=s, cs=cs: e.matmul(
                        bank[:], lhsT=wt[s][:, kc, :], rhs=hT[:, kc, cs], start=(kc == 0), stop=(kc == 7)),
                        reads=[(nm, s), "hT"], writes=[("pq", i2)])
                S.act(lambda e, bank=bank, i2=i2: e.activation(out=sq[i2][:], in_=bank[:], func=AF.Square),
                      reads=[("pq", i2)], writes=[("sq", i2)])
                S.dve(lambda e, bank=bank, i2=i2: e.tensor_copy(out=raw[i2][:], in_=bank[:]),
                      reads=[("pq", i2)], writes=[("raw", i2)])

            def proj_norm(ci, dst, dkey, gain, extra, ch):
                cs = slice(ch * 512, (ch + 1) * 512)
                i2 = ci % 2
                S.pe(lambda e, i2=i2: e.matmul(pss[:], lhsT=onesb[:], rhs=sq[i2][:], start=True, stop=True),
                     reads=["onesb", ("sq", i2)], writes=["pss"])
                S.act(lambda e, i2=i2: e.activation(out=rs[i2][:], in_=pss[:], func=AF.Ln, bias=EPS, scale=1.0 / 128.0),
                      reads=["pss"], writes=[("rs", i2)])
                S.act(lambda e, i2=i2, extra=extra: e.activation(out=rs[i2][:], in_=rs[i2][:], func=AF.Exp, scale=-0.5,
                                                                 bias=extra),
                      reads=[("rs", i2)], writes=[("rs", i2)])
                S.dve(lambda e, i2=i2, dst=dst, cs=cs, gain=gain, gi=gi: e.scalar_tensor_tensor(
                    out=dst[:, cs], in0=raw[i2][:], scalar=gain[:, gi:gi + 1], in1=rs[i2][:],
                    op0=ALU.mult, op1=ALU.mult), reads=[("raw", i2), ("rs", i2), "gq", "gk"], writes=[dkey])

            def v_group(b4):
                for bb in range(4):
                    blk = b4 * 4 + bb
                    r, b = divmod(blk, nb)
                    t0 = Dg * 128 * b + r
                    for kc in range(8):
                        S.pe(lambda e, kc=kc, bb=bb, t0=t0, Dg=Dg, s=s: e.matmul(
                            pvf[:, bb, :], lhsT=hT[:, kc, t0:t0 + 127 * Dg + 1:Dg], rhs=wv[s][:, kc, :],
                            start=(kc == 0), stop=(kc == 7)), reads=["hT", ("wv", s)], writes=["pvf"])
                S.act(lambda e, b4=b4: e.copy(out=V[:, b4 * 4:(b4 + 1) * 4, :], in_=pvf[:]),
                      reads=["pvf"], writes=["V"])

            plist = [(wq, "wq", qT, "qT", gq, LN_SCALE, ch) for ch in range(NCH)] + \
                    [(wk, "wk", kT, "kT", gk, 0.0, ch) for ch in range(NCH)]
            nvg = NTL // 4
            vdone = 0
            pend = None
            for ci, (wt, nm, dst, dkey, gain, extra, ch) in enumerate(plist):
                proj_mm(ci, wt, nm, ch)
                if pend is not None:
                    proj_norm(*pend)
                pend = (ci, dst, dkey, gain, extra, ch)
                want = ((ci + 1) * nvg) // len(plist)
                while vdone < want:
                    v_group(vdone)
                    vdone += 1
            proj_norm(*pend)
            while vdone < nvg:
                v_group(vdone)
                vdone += 1

            def scores(pos, r, kb):
                nq = 256 if kb + 1 < nb else 128
                k0 = Dg * 128 * kb + r
                pi = pos % 3
                bi = pos % 2
                bank = pst[bi]
                S.pe(lambda e, bank=bank, k0=k0, Dg=Dg, nq=nq: e.matmul(
                    bank[:, 0:nq], lhsT=kT[:, k0:k0 + 127 * Dg + 1:Dg], rhs=qT[:, k0:k0 + (nq - 1) * Dg + 1:Dg],
                    start=True, stop=False), reads=["kT", "qT"], writes=[("pq", bi)])
                S.pe(lambda e, bank=bank, nq=nq: e.matmul(
                    bank[:, 0:nq], lhsT=idb[:], rhs=mneg[:, 0:nq], start=False, stop=True),
                    reads=["idb", "mneg"], writes=[("pq", bi)])
                S.act(lambda e, bank=bank, pi=pi, nq=nq: e.activation(out=PT[pi][:, 0:nq], in_=bank[:, 0:nq], func=AF.Exp),
                      reads=[("pq", bi)], writes=[("PT", pi)])

            grp = [0]

            def pvacc(pos, r, qb):
                srcs = []
                if qb >= 1:
                    srcs.append((r * nb + qb - 1, (pos - 1) % 3, slice(128, 256)))
                srcs.append((r * nb + qb, pos % 3, slice(0, 128)))
                q4 = qb % 4
                g2 = grp[0] % 2
                for dst_ps, dkey, use_v in ((po[g2], ("po", g2), True), (pl[g2], ("pl", g2), False)):
                    for si, (vblk, pi, csl) in enumerate(srcs):
                        S.pe(lambda e, dst_ps=dst_ps, use_v=use_v, vblk=vblk, pi=pi, csl=csl, q4=q4, si=si, n=len(srcs): e.matmul(
                            dst_ps[:, q4, :], lhsT=(V[:, vblk, :] if use_v else onesb[:]), rhs=PT[pi][:, csl],
                            start=(si == 0), stop=(si == n - 1)),
                            reads=["V", "onesb", ("PT", pi)], writes=[dkey])
                if q4 == 3 or qb == nb - 1:
                    nqb = q4 + 1
                    qb0 = qb - q4
                    t0 = Dg * 128 * qb0 + r
                    tsl = slice(t0, t0 + (nqb * 128 - 1) * Dg + 1, Dg)
                    for dst_ps, dkey, acc, akey in ((po[g2], ("po", g2), oacc, "oacc"), (pl[g2], ("pl", g2), lacc, "lacc")):
                        src = dst_ps[:, 0:nqb, :].rearrange("p a b -> p (a b)")
                        if gi == 0:
                            S.dve(lambda e, src=src, acc=acc, tsl=tsl: e.tensor_copy(out=acc[:, tsl], in_=src),
                                  reads=[dkey], writes=[akey])
                        else:
                            S.dve(lambda e, src=src, acc=acc, tsl=tsl: e.tensor_tensor(
                                out=acc[:, tsl], in0=src, in1=acc[:, tsl], op=ALU.add),
                                reads=[dkey, akey], writes=[akey])
                    grp[0] += 1

            seq = [(r, kb) for r in range(Dg) for kb in range(nb)]
            prev = None
            for pos, (r, kb) in enumerate(seq):
                scores(pos, r, kb)
                if prev is not None:
                    pvacc(*prev)
                prev = (pos, r, kb)
            pvacc(*prev)
            if gi == 2:
                S.act(lambda e: e.activation(out=lacc[:], in_=lacc[:], func=AF.Ln), reads=["lacc"], writes=["lacc"])
                S.act(lambda e: e.activation(out=lacc[:], in_=lacc[:], func=AF.Exp, scale=-1.0), reads=["lacc"], writes=["lacc"])
                S.dve(lambda e: e.tensor_tensor(out=onb[:], in0=oacc[:], in1=lacc[:], op=ALU.mult),
                      reads=["oacc", "lacc"], writes=["onb"])
                S.dma("sp", "ost", onT[h * 128:(h + 1) * 128, :], onb[:], reads=["onb"])
        S.emit(final_chans=["ost"])


def phase_swa_out(nc, x_src, x_dst, onT, w_out, S_len):
    import contextlib
    NTL = S_len // 128
    with contextlib.ExitStack() as es:
        C = Ctx(nc, es)
        S = C.S
        wout_sb = C.sb("wout", [128, 8, D], BF16)
        ot = [C.sb("ot%d" % i, [128, 8, 128], BF16) for i in range(2)]
        xr = [C.sb("xr%d" % i, [128, D], F32) for i in range(2)]
        pp = [C.ps("pp%d" % i, [128, 512], F32) for i in range(4)]
        S.psum_keys.update([("pp", i) for i in range(4)])
        S.dma("pool", "w_out", wout_sb[:], w_out.rearrange("(kc p) n -> p kc n", p=128), writes=["wout"])
        onT_v = onT.rearrange("(h p) t -> p h t", p=128)
        for j in range(NTL):
            s = j % 2
            S.dma("sp", "ot%d" % s, ot[s][:], onT_v[:, :, j * 128:(j + 1) * 128], writes=[("ot", s)])
            S.dma("sp", "xr%d" % s, xr[s][:], x_src[j * 128:(j + 1) * 128, :], writes=[("xr", s)])
            for half in range(2):
                bi = (2 * j + half) % 4
                bank = pp[bi]
                for c8 in range(8):
                    S.pe(lambda e, c8=c8, half=half, bank=bank, s=s: e.matmul(
                        bank[:], lhsT=ot[s][:, c8, :], rhs=wout_sb[:, c8, half * 512:(half + 1) * 512],
                        start=(c8 == 0), stop=(c8 == 7)), reads=[("ot", s), "wout"], writes=[("pp", bi)])
                S.dve(lambda e, s=s, half=half, bank=bank: e.tensor_tensor(
                    out=xr[s][:, half * 512:(half + 1) * 512], in0=bank[:],
                    in1=xr[s][:, half * 512:(half + 1) * 512], op=ALU.add),
                    reads=[("pp", bi), ("xr", s)], writes=[("xr", s)])
            S.dma("pool", "xs%d" % s, x_dst[j * 128:(j + 1) * 128, :], xr[s][:], reads=[("xr", s)])
        S.emit(final_chans=["xs0", "xs1"])


DEPTH = 4


def build_program(S_len):
    nc = bass.Bass("TRN2", target_bir_lowering=False)

    def inp(name, shape):
        return nc.dram_tensor(name, shape, F32, kind="ExternalInput").ap()

    x = inp("x", [S_len, D])
    norm_mix = inp("norm_mix", [4, 128, 8])
    norm_mlp = inp("norm_mlp", [4, 128, 8])
    gla_w_in = inp("gla_w_in", [2, D, GLA_IN])
    gla_w_gate = inp("gla_w_gate_up", [2, 16, 512])
    gla_b_gate = inp("gla_b_gate", [2, 1, 512])
    gla_g_out = inp("gla_g_out", [2, 1, 1024])
    gla_w_out = inp("gla_w_out", [2, D, D])
    swa_w_qkv = inp("swa_w_qkv", [2, D, SWA_IN])
    swa_g_q = inp("swa_g_q", [2, 128, 3])
    swa_g_k = inp("swa_g_k", [2, 128, 3])
    swa_w_out = inp("swa_w_out", [2, D, D])
    mlp_w_up = inp("mlp_w_up", [4, D, DFF])
    mlp_w_down = inp("mlp_w_down", [4, DFF, D])
    ident = inp("c_ident", [128, 128])
    tri = inp("c_tri", [128, 128])
    trineg = inp("c_trineg", [128, 128])
    maskneg = inp("c_maskneg", [128, 256])
    out = nc.dram_tensor("out", [S_len, D], F32, kind="ExternalOutput").ap()
    onT = nc.dram_tensor("onT_scratch", [D, S_len], BF16).ap()
    src = x
    for i in range(DEPTH):
        j = i // 2
        if i % 2 == 0:
            phase_gla(nc, src, out, norm_mix[i], gla_w_in[j], gla_w_gate[j], gla_b_gate[j], gla_g_out[j],
                      gla_w_out[j], ident, tri, trineg, S_len)
        else:
            phase_swa_attn(nc, src, onT, norm_mix[i], swa_w_qkv[j], swa_g_q[j], swa_g_k[j], ident, maskneg, S_len)
            phase_swa_out(nc, src, out, onT, swa_w_out[j], S_len)
        src = out
        phase_mlp(nc, src, out, norm_mlp[i], mlp_w_up[i], mlp_w_down[i], ident, S_len)
    return nc


def make_consts():
    jj, ii = np.meshgrid(np.arange(128), np.arange(128), indexing="ij")
    tri = (jj <= ii).astype(np.float32)
    i2, c2 = np.meshgrid(np.arange(128), np.arange(256), indexing="ij")
    mk = np.where((c2 >= i2) & (c2 <= i2 + 128), 0.0, -30000.0).astype(np.float32)
    return {"c_ident": np.eye(128, dtype=np.float32), "c_tri": tri, "c_trineg": (-tri / 16.0).astype(np.float32),
            "c_maskneg": mk}


def layout_inputs(inputs):
    f = lambda a: np.ascontiguousarray(np.asarray(a, dtype=np.float32))
    shared = {
        "norm_mix": f(np.asarray(inputs["norm_mix"]).reshape(-1, 8, 128).transpose(0, 2, 1)),
        "norm_mlp": f(np.asarray(inputs["norm_mlp"]).reshape(-1, 8, 128).transpose(0, 2, 1)),
        "gla_w_in": f(inputs["gla_w_in"]),
        "gla_w_gate_up": f(inputs["gla_w_gate_up"]),
        "gla_b_gate": f(np.asarray(inputs["gla_b_gate"]).reshape(2, 1, 512)),
        "gla_g_out": f(np.asarray(inputs["gla_g_out"]).reshape(2, 1, 1024)),
        "gla_w_out": f(inputs["gla_w_out"]),
        "swa_w_qkv": f(inputs["swa_w_qkv"]),
        "swa_g_q": f(np.asarray(inputs["swa_g_q"]).transpose(0, 2, 1)),
        "swa_g_k": f(np.asarray(inputs["swa_g_k"]).transpose(0, 2, 1)),
        "swa_w_out": f(inputs["swa_w_out"]),
        "mlp_w_up": f(inputs["mlp_w_up"]),
        "mlp_w_down": f(inputs["mlp_w_down"]),
    }
    shared.update(make_consts())
    return shared


def kernel(**inputs):
    x = np.asarray(inputs["x"], dtype=np.float32)
    B, S_len, _ = x.shape
    shared = layout_inputs(inputs)
    nc = build_program(S_len)
    in_maps = []
    for b in range(B):
        m = dict(shared)
        m["x"] = np.ascontiguousarray(x[b])
        in_maps.append(m)
    res = run_bass_kernel_spmd(nc, in_maps, core_ids=list(range(B)))
    return np.stack([np.asarray(r["out"], dtype=np.float32) for r in res.results], axis=0)
```
